# Optimizing a Trainium2 kernel written in Bass

```python
import math
import jax, jax.numpy as jnp
from jax import lax
import numpy as np

D_MODEL = 1024
BATCH = 4
SEQ = 4096
DEPTH = 2

PLE_DIM = 256
CHUNK = 64
D_RNN = D_MODEL // 2
RNN_BLOCKS = 8
RNN_BLOCK_DIM = D_RNN // RNN_BLOCKS
CONV_WIDTH = 4
LRU_C = 8.0
HEAD_DIM = 64
N_ATT_HEADS = (D_MODEL - D_RNN) // HEAD_DIM
D_ATT = N_ATT_HEADS * HEAD_DIM
D_MIX = D_RNN + D_ATT
D_IN = 2 * D_RNN + 3 * D_ATT + N_ATT_HEADS
Q_BLOCK = 128
N_GROUPS = 4
EXPERTS_PER_GROUP = 8
N_EXPERTS = N_GROUPS * EXPERTS_PER_GROUP
TOP_K = 2
D_EXPERT = D_MODEL // 2
EPS = 1e-6

kernel_name = "hybrid_rglru_fox_hmoe_block"


def rmsnorm(x, g):
    xf = x.astype(jnp.float32)
    y = xf * lax.rsqrt(jnp.mean(xf * xf, axis=-1, keepdims=True) + EPS)
    return (y * g.astype(jnp.float32)).astype(x.dtype)


def causal_depthwise_conv(x, w, b):
    c = x.shape[-1]
    y = lax.conv_general_dilated(
        x, w[:, None, :].astype(x.dtype), window_strides=(1,), padding=[(CONV_WIDTH - 1, 0)],
        dimension_numbers=("NWC", "WIO", "NWC"), feature_group_count=c)
    return y + b.astype(x.dtype)


def rg_lru(xc, w_r, b_r, w_i, b_i, lam):
    B, S, _ = xc.shape
    xb = xc.reshape(B, S, RNN_BLOCKS, RNN_BLOCK_DIM)
    r = jax.nn.sigmoid((jnp.einsum('bshi,hij->bshj', xb, w_r).reshape(B, S, D_RNN) + b_r).astype(jnp.float32))
    i = jax.nn.sigmoid((jnp.einsum('bshi,hij->bshj', xb, w_i).reshape(B, S, D_RNN) + b_i).astype(jnp.float32))
    log_a = -LRU_C * r * jax.nn.softplus(-lam.astype(jnp.float32))
    a = jnp.exp(log_a)
    u = jnp.sqrt(-jnp.expm1(2.0 * log_a)) * (i * xc.astype(jnp.float32))

    def combine(left, right):
        a1, b1 = left
        a2, b2 = right
        return a1 * a2, a2 * b1 + b2

    _, h = lax.associative_scan(combine, (a, u), axis=1)
    return h.astype(xc.dtype)


def forgetting_attention(q, k, v, cum_logf):
    S = q.shape[2]
    scale = 1.0 / math.sqrt(HEAD_DIM)
    outs = []
    for qb in range(S // Q_BLOCK):
        lo, hi = qb * Q_BLOCK, (qb + 1) * Q_BLOCK
        s = jnp.einsum('bhqd,bhkd->bhqk', q[:, :, lo:hi], k[:, :, :hi]).astype(jnp.float32) * scale
        s = s + cum_logf[:, :, lo:hi, None] - cum_logf[:, :, None, :hi]
        mask = jnp.arange(hi)[None, :] <= jnp.arange(lo, hi)[:, None]
        s = jnp.where(mask, s, -jnp.inf)
        pr = jax.nn.softmax(s, axis=-1).astype(v.dtype)
        outs.append(jnp.einsum('bhqk,bhkd->bhqd', pr, v[:, :, :hi]))
    return jnp.concatenate(outs, axis=2)


def hierarchical_moe(n, w_group, b_group, w_router, b_router, w_gate, w_up, w_down):
    B, S, D = n.shape
    T = B * S
    xf = n.reshape(T, D)
    g_prob = jax.nn.softmax((xf @ w_group).astype(jnp.float32) + b_group.astype(jnp.float32), axis=-1)
    g_p, g_idx = lax.top_k(g_prob, 1)
    e_logits = ((xf @ w_router).astype(jnp.float32) + b_router.astype(jnp.float32)).reshape(T, N_GROUPS, EXPERTS_PER_GROUP)
    e_logits = jnp.take_along_axis(e_logits, g_idx[:, :, None], axis=1)[:, 0]
    e_top, e_loc = lax.top_k(e_logits, TOP_K)
    e_w = jax.nn.softmax(e_top, axis=-1) * g_p
    flat_id = (g_idx * EXPERTS_PER_GROUP + e_loc).reshape(-1)
    order = jnp.argsort(flat_id)
    tok = order // TOP_K
    sizes = jnp.bincount(flat_id, length=N_EXPERTS).astype(jnp.int32)
    xs = xf[tok]
    hg = lax.ragged_dot(xs, w_gate, sizes)
    hu = lax.ragged_dot(xs, w_up, sizes)
    ys = lax.ragged_dot(jax.nn.silu(hg) * hu, w_down, sizes)
    ws = e_w.reshape(-1)[order].astype(ys.dtype)
    y = jnp.zeros_like(xf).at[tok].add(ys * ws[:, None])
    return y.reshape(B, S, D)


def setup_inputs(seed: int = 0) -> dict:
    key = jax.random.key(seed)
    ks = iter(jax.random.split(key, 40))
    f32 = jnp.float32

    def nrm(shape, scale):
        return jax.random.normal(next(ks), shape, f32) * scale

    def gain(shape):
        return 1.0 + 0.01 * jax.random.normal(next(ks), shape, f32)

    a8 = jax.random.uniform(next(ks), (DEPTH, D_RNN), f32, 0.9, 0.999)
    a_base = a8 ** (1.0 / LRU_C)
    lru_lambda = jnp.log(a_base) - jnp.log1p(-a_base)

    return {
        "x": jax.random.normal(next(ks), (BATCH, SEQ, D_MODEL), f32),
        "p": jax.random.normal(next(ks), (DEPTH, BATCH, SEQ, PLE_DIM), f32),
        "mix_norm": gain((DEPTH, D_MODEL)),
        "w_in": nrm((DEPTH, D_MODEL, D_IN), D_MODEL ** -0.5),
        "b_forget": jax.random.uniform(next(ks), (DEPTH, N_ATT_HEADS), f32, 1.0, 4.0),
        "conv_w": nrm((DEPTH, CONV_WIDTH, D_RNN), CONV_WIDTH ** -0.5),
        "conv_b": nrm((DEPTH, D_RNN), 0.01),
        "w_rgate": nrm((DEPTH, RNN_BLOCKS, RNN_BLOCK_DIM, RNN_BLOCK_DIM), RNN_BLOCK_DIM ** -0.5),
        "b_rgate": nrm((DEPTH, D_RNN), 0.01),
        "w_igate": nrm((DEPTH, RNN_BLOCKS, RNN_BLOCK_DIM, RNN_BLOCK_DIM), RNN_BLOCK_DIM ** -0.5),
        "b_igate": nrm((DEPTH, D_RNN), 0.01),
        "lru_lambda": lru_lambda,
        "q_norm": gain((DEPTH, HEAD_DIM)),
        "k_norm": gain((DEPTH, HEAD_DIM)),
        "lru_out_norm": gain((DEPTH, D_RNN)),
        "att_out_norm": gain((DEPTH, D_ATT)),
        "w_out": nrm((DEPTH, D_MIX, D_MODEL), D_MIX ** -0.5),
        "ffn_norm": gain((DEPTH, D_MODEL)),
        "w_group": nrm((DEPTH, D_MODEL, N_GROUPS), D_MODEL ** -0.5),
        "b_group": nrm((DEPTH, N_GROUPS), 0.01),
        "w_router": nrm((DEPTH, D_MODEL, N_EXPERTS), D_MODEL ** -0.5),
        "b_router": nrm((DEPTH, N_EXPERTS), 0.01),
        "w_exp_gate": nrm((DEPTH, N_EXPERTS, D_MODEL, D_EXPERT), D_MODEL ** -0.5),
        "w_exp_up": nrm((DEPTH, N_EXPERTS, D_MODEL, D_EXPERT), D_MODEL ** -0.5),
        "w_exp_down": nrm((DEPTH, N_EXPERTS, D_EXPERT, D_MODEL), D_EXPERT ** -0.5),
        "ple_norm": gain((DEPTH, D_MODEL)),
        "w_ple_gate": nrm((DEPTH, D_MODEL, D_MODEL), D_MODEL ** -0.5),
        "b_ple_gate": nrm((DEPTH, D_MODEL), 0.01),
        "w_ple_proj": nrm((DEPTH, PLE_DIM, D_MODEL), PLE_DIM ** -0.5),
    }


def reference(x, p, mix_norm, w_in, b_forget, conv_w, conv_b, w_rgate, b_rgate, w_igate, b_igate,
              lru_lambda, q_norm, k_norm, lru_out_norm, att_out_norm, w_out, ffn_norm,
              w_group, b_group, w_router, b_router, w_exp_gate, w_exp_up, w_exp_down,
              ple_norm, w_ple_gate, b_ple_gate, w_ple_proj):
    B, S, _ = x.shape
    splits = np.cumsum([D_RNN, D_RNN, D_ATT, D_ATT, D_ATT]).tolist()
    for i in range(DEPTH):
        n = rmsnorm(x, mix_norm[i])
        z = n @ w_in[i]
        z_x, z_gate, z_q, z_k, z_v, z_f = jnp.split(z, splits, axis=-1)

        xc = causal_depthwise_conv(z_x, conv_w[i], conv_b[i])
        h = rg_lru(xc, w_rgate[i], b_rgate[i], w_igate[i], b_igate[i], lru_lambda[i])
        y_rnn = h * jax.nn.gelu(z_gate)

        q = rmsnorm(z_q.reshape(B, S, N_ATT_HEADS, HEAD_DIM), q_norm[i]).transpose(0, 2, 1, 3)
        k = rmsnorm(z_k.reshape(B, S, N_ATT_HEADS, HEAD_DIM), k_norm[i]).transpose(0, 2, 1, 3)
        v = z_v.reshape(B, S, N_ATT_HEADS, HEAD_DIM).transpose(0, 2, 1, 3)
        logf = jax.nn.log_sigmoid(z_f.astype(jnp.float32) + b_forget[i].astype(jnp.float32))
        cum_logf = jnp.cumsum(logf, axis=1).transpose(0, 2, 1)
        o = forgetting_attention(q, k, v, cum_logf)
        y_att = o.transpose(0, 2, 1, 3).reshape(B, S, D_ATT)

        y_mix = jnp.concatenate([rmsnorm(y_rnn, lru_out_norm[i]), rmsnorm(y_att, att_out_norm[i])], axis=-1)
        x = x + y_mix @ w_out[i]

        x = x + hierarchical_moe(rmsnorm(x, ffn_norm[i]), w_group[i], b_group[i], w_router[i], b_router[i],
                                 w_exp_gate[i], w_exp_up[i], w_exp_down[i])

        gate = jax.nn.sigmoid(rmsnorm(x, ple_norm[i]) @ w_ple_gate[i] + b_ple_gate[i])
        x = x + (p[i] @ w_ple_proj[i]) * gate
    return x
```

```python
import numpy as np
from contextlib import ExitStack
import concourse.bass as bass
import concourse.mybir as mybir
from concourse.bass_utils import run_bass_kernel_spmd

F32 = mybir.dt.float32
BF16 = mybir.dt.bfloat16
I32 = mybir.dt.int32
U32 = mybir.dt.uint32
AF = mybir.ActivationFunctionType
ALU = mybir.AluOpType
AX = mybir.AxisListType

ENGS = ("sync", "scalar", "vector", "gpsimd", "tensor")
NPOOL = 56
NPOOL_HW = 28
SAME_ENGINE_SYNC = True


def _conf(a, b):
    n = min(len(a), len(b))
    return a[:n] == b[:n]


class _Rec:
    def __init__(self):
        self.call = None

    def __getattr__(self, name):
        def f(*a, **k):
            assert self.call is None
            self.call = (name, a, k)
            return self
        return f


class Sems:
    def __init__(self, nc):
        self.stack = ExitStack()
        st = self.stack
        self.esem = {e: st.enter_context(nc.semaphore(f"es_{e}")) for e in ENGS}
        self.pool = [st.enter_context(nc.semaphore(f"dp_{i}")) for i in range(NPOOL)]
        self.cc = st.enter_context(nc.semaphore("ccs"))
        self.bar = st.enter_context(nc.semaphore("bars"))
        self.ecount = {e: 0 for e in ENGS}
        self.pcount = [0] * NPOOL
        self.cccount = 0
        self.nphase = 0
        self.ndma = 0
        self.ndma_sw = 0

    def close(self):
        self.stack.close()


class Prog:
    def __init__(self, nc, sems=None):
        self.nc = nc
        self.sems = sems
        self.ops = []
        self.track = {}
        self.stack = ExitStack()
        self.ntens = 0

    def sb(self, shape, dt, name=None):
        self.ntens += 1
        name = name or f"t{self.ntens}"
        return self.stack.enter_context(self.nc.sbuf_tensor(name, list(shape), dt))

    def ps(self, shape, dt, name=None):
        self.ntens += 1
        name = name or f"p{self.ntens}"
        return self.stack.enter_context(self.nc.psum_tensor(name, list(shape), dt))

    def _deps(self, reads, writes):
        deps = set()
        for k in reads:
            k = k if isinstance(k, tuple) else (k,)
            for sk, ent in self.track.get(k[0], {}).items():
                if _conf(sk, k) and ent[0] is not None:
                    deps.add(ent[0])
        for k in writes:
            k = k if isinstance(k, tuple) else (k,)
            for sk, ent in self.track.get(k[0], {}).items():
                if _conf(sk, k):
                    if ent[0] is not None:
                        deps.add(ent[0])
                    deps.update(ent[1])
        return deps

    def _commit(self, idx, reads, writes):
        for k in reads:
            k = k if isinstance(k, tuple) else (k,)
            d = self.track.setdefault(k[0], {})
            if k not in d:
                lw = None
                for sk, ent in d.items():
                    if _conf(sk, k) and ent[0] is not None:
                        lw = ent[0] if lw is None else max(lw, ent[0])
                d[k] = [lw, []]
            d[k][1].append(idx)
        for k in writes:
            k = k if isinstance(k, tuple) else (k,)
            d = self.track.setdefault(k[0], {})
            for sk in [sk for sk in d if _conf(sk, k) and sk != k]:
                if len(sk) > len(k):
                    del d[sk]
                else:
                    d[sk] = [idx, []]
            d[k] = [idx, []]

    def capture(self, automark=False):
        self._cap = []
        self._automark = automark

    def hold(self):
        self._hold = getattr(self, "_hold", 0) + 1

    def release(self):
        self._hold -= 1
        if self._hold == 0:
            self.mark()

    def mark(self):
        if getattr(self, "_cap", None) is not None:
            self._cap.append(None)

    def end_capture(self):
        c, self._cap = self._cap, None
        return c

    def replay(self, streams):
        units = []
        for st in streams:
            us, cur = [], []
            for it in st:
                if it is None:
                    if cur:
                        us.append(cur)
                    cur = []
                else:
                    cur.append(it)
            if cur:
                us.append(cur)
            units.append(us)
        n = max(len(u) for u in units)
        for k in range(n):
            for us in units:
                if k < len(us):
                    for (eng, fn, reads, writes, dma, cc) in us[k]:
                        self._op_now(eng, fn, reads, writes, dma, cc)

    def op(self, eng, fn, reads=(), writes=(), dma=False, cc=False):
        rec = _Rec()
        fn(rec)
        assert rec.call is not None
        call = rec.call
        fn = lambda e, call=call: getattr(e, call[0])(*call[1], **call[2])
        if getattr(self, "_cap", None) is not None:
            self._cap.append((eng, fn, list(reads), list(writes), dma, cc))
            if getattr(self, "_automark", False) and not getattr(self, "_hold", 0):
                self._cap.append(None)
            return None
        return self._op_now(eng, fn, reads, writes, dma, cc)

    def _op_now(self, eng, fn, reads=(), writes=(), dma=False, cc=False):
        idx = len(self.ops)
        deps = self._deps(reads, writes)
        self._commit(idx, reads, writes)
        if getattr(self, "serial", False) and idx > 0:
            deps.add(idx - 1)
        if cc:
            prev = getattr(self, "_last_cc", None)
            if prev is not None:
                deps.add(prev)
            self._last_cc = idx
        self.ops.append(dict(eng=eng, fn=fn, deps=sorted(deps), dma=dma, idx=idx, cc=cc))
        return idx

    def dma(self, eng, out, in_, reads=(), writes=(), **kw):
        return self.op(eng, lambda e: e.dma_start(out=out, in_=in_, **kw), reads, writes, dma=True)

    def emit(self):
        nc = self.nc
        ops = self.ops
        has_dep = [False] * len(ops)
        for o in ops:
            for d in o["deps"]:
                if SAME_ENGINE_SYNC or ops[d]["eng"] != o["eng"] or ops[d]["dma"]:
                    has_dep[d] = True
        own = self.sems is None
        sems = self.sems if self.sems is not None else Sems(nc)
        esem, pool, ecount, pcount = sems.esem, sems.pool, sems.ecount, sems.pcount
        pool_prev = [None] * NPOOL
        sems.nphase += 1
        for o in ops:
            if o["cc"]:
                sems.cccount += 1
                o["sig"] = (sems.cc, sems.cccount, ("cc",))
                o["pool_prev"] = None
            elif o["dma"]:
                if o["eng"] == "gpsimd":
                    s = NPOOL_HW + sems.ndma_sw % (NPOOL - NPOOL_HW)
                    sems.ndma_sw += 1
                else:
                    s = sems.ndma % NPOOL_HW
                    sems.ndma += 1
                pcount[s] += 16
                o["sig"] = (pool[s], pcount[s], ("p", s))
                o["pool_prev"] = pool_prev[s]
                pool_prev[s] = o["idx"]
            elif has_dep[o["idx"]] :
                ecount[o["eng"]] += 1
                o["sig"] = (esem[o["eng"]], ecount[o["eng"]], ("e", o["eng"]))
            else:
                o["sig"] = None
        per = {e: [o for o in ops if o["eng"] == e] for e in ENGS}

        def run(engname, eng):
            waited = {}
            for o in per[engname]:
                deps = list(o["deps"])
                if o["dma"] and o["pool_prev"] is not None:
                    deps.append(o["pool_prev"])
                need = {}
                for d in deps:
                    od = ops[d]
                    if not od["dma"] and not od["cc"] and od["eng"] == engname and not SAME_ENGINE_SYNC:
                        continue
                    if not od["dma"] and not od["cc"] and od["eng"] == "tensor" and engname == "tensor":
                        continue
                    sem, val, key = od["sig"]
                    if waited.get(key, 0) >= val:
                        continue
                    if key not in need or need[key][1] < val:
                        need[key] = (sem, val)
                for key, (sem, val) in need.items():
                    eng.wait_ge(sem, val)
                    waited[key] = val
                ins = o["fn"](eng)
                if o["sig"] is not None:
                    sem, val, key = o["sig"]
                    ins.then_inc(sem, 16 if (o["dma"] and not o["cc"]) else 1)
            for s_ in range(NPOOL):
                if pcount[s_] > 0:
                    eng.wait_ge(pool[s_], pcount[s_])
            if sems.cccount > 0:
                eng.wait_ge(sems.cc, sems.cccount)
            eng.drain().then_inc(sems.bar, 1)
            eng.wait_ge(sems.bar, 5 * sems.nphase)

        with nc.Block() as block:
            @block.sync
            def _(e):
                run("sync", e)

            @block.scalar
            def _(e):
                run("scalar", e)

            @block.vector
            def _(e):
                run("vector", e)

            @block.gpsimd
            def _(e):
                run("gpsimd", e)

            @block.tensor
            def _(e):
                run("tensor", e)
        self.stack.close()
        if own:
            sems.close()

    def finish_wait(self, eng, keys):
        return self.op(eng, lambda e: e.nop(), reads=keys, writes=())

NPV = 24
S = 4096
NCHUNK = 8
EPS = 1e-6


def mixer_phase(P, nc, d, pfx="m", stop=99, nchunk=NCHUNK):
    K = lambda *a: (pfx,) + a

    def key(*a):
        return (pfx + "_" + str(a[0]),) + tuple(a[1:])

    sb = lambda shape, dt, name: P.sb(shape, dt, pfx + "_" + name)
    gmix = sb([128, 1024], F32, "gmix")
    wb = sb([128, 8, 1284], BF16, "wb")
    pv = sb([128, NPV], F32, "pv")
    wr = sb([128, 2, 128], BF16, "wr")
    wi = sb([128, 2, 128], BF16, "wi")
    bfb = sb([128, 4], F32, "bfb")
    ident_b = sb([128, 128], BF16, "ident_b")
    tri_b = sb([128, 128], BF16, "tri_b")
    tri_f = sb([128, 128], F32, "tri_f")
    ones_f = sb([128, 128], F32, "ones_f")
    ones_b = sb([128, 128], BF16, "ones_b")
    ones_bd = sb([128, 128], BF16, "ones_bd")
    eps = sb([128, 1], F32, "eps")
    one1 = sb([128, 1], F32, "one1")
    cl = sb([128, 2, 2], F32, "cl")
    gq8 = sb([128, 1], F32, "gq8")
    xt = [sb([128, 1024], F32, f"xt{i}") for i in range(2)]
    st = [sb([128, 4], F32, f"st{i}") for i in range(2)]
    nb1 = sb([128, 1024], BF16, "nb0")
    nb = [nb1, nb1]
    nT = [sb([128, 8, 512], BF16, f"nT{i}") for i in range(2)]
    zxb = [sb([128, 515], F32, f"zxb{c}") for c in range(2)]
    xc = [sb([128, 512], F32, f"xc{i}") for i in range(2)]
    xcb = [sb([128, 512], BF16, f"xcb{i}") for i in range(2)]
    rr = [sb([128, 512], F32, f"rr{i}") for i in range(2)]
    ig = [sb([128, 512], F32, f"ig{i}") for i in range(2)]
    aa = [sb([128, 512], F32, f"aa{i}") for i in range(2)]
    a2 = [sb([128, 512], F32, f"a2{i}") for i in range(2)]
    sq_ = [sb([128, 512], F32, f"sq_{i}") for i in range(2)]
    uu = [sb([128, 512], F32, f"uu{i}") for i in range(2)]
    hh = [sb([128, 512], F32, f"hh{i}") for i in range(2)]
    hst = sb([128, 2], F32, "hst")
    gg = [sb([128, 512], F32, f"gg{i}") for i in range(2)]
    yy = [sb([128, 512], F32, f"yy{i}") for i in range(2)]
    ysq = [sb([128, 512], BF16, f"ysq{c}") for c in range(2)]
    ygb = [sb([128, 512], BF16, f"ygb{c}") for c in range(2)]
    qsq = [sb([128, 512], BF16, f"qsq{i}") for i in range(2)]
    qlr = [sb([128, 512], F32, f"qlr{i}") for i in range(2)]
    qrs = [sb([128, 512], F32, f"qrs{i}") for i in range(2)]
    q_aug = [sb([65, 4, 512], BF16, f"qaug{i}") for i in range(2)]
    k_aug = sb([65, 4, S], BF16, "kaug")
    Vp = sb([128, 32, 4, 128], BF16, "Vp")
    den_sb = sb([128, 512], F32, "den_sb")
    dhi = sb([128, 512], BF16, "dhi")
    dlo = sb([128, 512], BF16, "dlo")
    shb = sb([128, 64], BF16, "shb")
    nlb = sb([128, 4, 4, 65], BF16, "nlb")
    low_b = sb([128, 128], BF16, "low_b")
    ft = sb([128, 4], F32, "ft")
    fe = sb([128, 4], F32, "fe")
    nl128 = sb([128, 128], F32, "nl128")
    nl = nl128[:, 0:4]
    carry = sb([128, 4], F32, "carry")
    nc_tok = sb([128, 32, 4], F32, "nc_tok")
    cref = [sb([128, 4], F32, f"cref{i}") for i in range(2)]
    bias_t = [sb([128, 32, 4], F32, f"bias{i}") for i in range(2)]
    Pt = [sb([128, 512], BF16, f"Pt{i}") for i in range(4)]
    yat = sb([64, 512], F32, "yat")
    asq = [sb([64, 512], BF16, f"asq{h}") for h in range(4)]
    agb = [sb([64, 512], BF16, f"agb{h}") for h in range(2)]
    sso = sb([128, 4, 2], F32, "sso")
    pa = [P.ps([128, 512], F32, pfx + f"_pa{i}") for i in range(2)]
    dps = P.ps([128, 512], F32, pfx + "_dps")
    pT = P.ps([128, 8, 128], BF16, pfx + "_pT")
    sps = [P.ps([128, 512], F32, pfx + f"_sps{i}") for i in range(2)]
    ops_ = P.ps([128, 512], F32, pfx + "_ops")
    misc = P.ps([128, 512], F32, pfx + "_misc")
    pa_i = [0]

    pa_pool = [(pa[0], key("pa", 0)), (pa[1], key("pa", 1)), (dps, key("dps")), (sps[0], key("sps", 0)),
               (sps[1], key("sps", 1)), (ops_, key("ops"))]

    s_pool = [(sps[0], key("sps", 0)), (sps[1], key("sps", 1)), (pa[0], key("pa", 0)), (pa[1], key("pa", 1))]

    def next_pa():
        i = pa_i[0] % len(pa_pool)
        pa_i[0] += 1
        return pa_pool[i]

    V, SC, G, T = "vector", "scalar", "gpsimd", "tensor"

    P.dma("sync", gmix[:], d["mixn"].partition_broadcast(128), writes=[key("gmix")])
    P.dma("sync", pv[:], d["pv"][:, :], writes=[key("pv")])
    P.dma("sync", bfb[:], d["bf"].partition_broadcast(128), writes=[key("bfb")])
    P.dma("gpsimd", wb[:], d["win"].rearrange("(c p) n -> p c n", p=128), writes=[key("wb")])
    P.dma("gpsimd", wr[:], d["wr_bd"].rearrange("c p n -> p c n"), writes=[key("wr")])
    P.dma("gpsimd", wi[:], d["wi_bd"].rearrange("c p n -> p c n"), writes=[key("wi")])
    P.op(V, lambda e: e.memset(eps[:], EPS), writes=[key("eps")])
    P.op(V, lambda e: e.memset(one1[:], 1.0), writes=[key("one1")])
    P.op(V, lambda e: e.memset(ones_f[:], 1.0), writes=[key("ones_f")])
    P.op(V, lambda e: e.memset(ones_b[:], 1.0), writes=[key("ones_b")])
    P.op(V, lambda e: e.memset(ones_bd[:], 0.0), writes=[key("ones_bd")])
    P.op(V, lambda e: e.memset(ones_bd[0:64, 0:64], 1.0), writes=[key("ones_bd")])
    P.op(V, lambda e: e.memset(ones_bd[64:128, 64:128], 1.0), writes=[key("ones_bd")])
    P.op(G, lambda e: e.affine_select(out=ident_b[:], in_=ones_b[:], pattern=[[1, 128]], compare_op=ALU.is_equal,
                                      fill=0.0, base=0, channel_multiplier=-1),
         reads=[key("ones_b")], writes=[key("ident_b")])
    P.op(G, lambda e: e.affine_select(out=tri_b[:], in_=ones_b[:], pattern=[[1, 128]], compare_op=ALU.is_ge,
                                      fill=0.0, base=0, channel_multiplier=-1),
         reads=[key("ones_b")], writes=[key("tri_b")])
    P.op(G, lambda e: e.affine_select(out=tri_f[:], in_=ones_f[:], pattern=[[1, 128]], compare_op=ALU.is_ge,
                                      fill=0.0, base=0, channel_multiplier=-1),
         reads=[key("ones_f")], writes=[key("tri_f")])
    P.op(V, lambda e: e.memset(k_aug[64:65, :, :], 1.0), writes=[key("kaug", "aug")])
    P.op(G, lambda e: e.affine_select(out=low_b[:], in_=ones_b[:], pattern=[[-1, 128]], compare_op=ALU.is_gt,
                                      fill=0.0, base=0, channel_multiplier=1),
         reads=[key("ones_b")], writes=[key("low_b")])
    P.op(V, lambda e: e.memset(carry[:], 0.0), writes=[key("carry")])
    P.op(G, lambda e: e.memset(Vp[:, :, :, 64:128], 1.0), writes=[key("Vp", "ones")])
    P.op(V, lambda e: e.memset(dhi[:], 0.0), writes=[key("dhi")])
    P.op(V, lambda e: e.memset(dlo[:], 0.0), writes=[key("dlo")])
    P.op(G, lambda e: e.affine_select(out=shb[:], in_=ones_b[:, 0:64], pattern=[[-1, 64]], compare_op=ALU.is_equal,
                                      fill=0.0, base=-64, channel_multiplier=1),
         reads=[key("ones_b")], writes=[key("shb")])
    P.op(V, lambda e: e.memset(nlb[:], 0.0), writes=[key("nlb")])
    P.op(V, lambda e: e.memset(nl128[:], 0.0), writes=[key("nl")])
    P.op(V, lambda e: e.memset(hst[:], 0.0), writes=[key("hst")])
    for c in range(2):
        P.op(V, lambda e, c=c: e.memset(zxb[c][:, 0:3], 0.0), writes=[key("zxb", c, "halo")])
    P.op(SC, lambda e: e.activation(out=cl[:, :, 0], in_=pv[:, 14:16], func=AF.Exp, scale=-1.0),
         reads=[key("pv")], writes=[key("cl")])
    P.op(SC, lambda e: e.activation(out=cl[:, :, 1], in_=cl[:, :, 0], func=AF.Ln, bias=one1[:]),
         reads=[key("cl"), key("one1")], writes=[key("cl")])
    P.op(V, lambda e: e.tensor_scalar(out=cl[:, :, 0], in0=cl[:, :, 1], scalar1=-8.0, scalar2=None, op0=ALU.mult),
         reads=[key("cl")], writes=[key("cl")])
    P.op(V, lambda e: e.tensor_scalar(out=cl[:, :, 1], in0=cl[:, :, 1], scalar1=-16.0, scalar2=None, op0=ALU.mult),
         reads=[key("cl")], writes=[key("cl")])
    P.op(V, lambda e: e.tensor_scalar(out=gq8[:], in0=pv[:, 18:19], scalar1=0.125, scalar2=None, op0=ALU.mult),
         reads=[key("pv")], writes=[key("gq8")])

    def pvc(col):
        return pv[:, col:col + 1]

    if stop <= 0:
        return [key('gmix')]
    for i in range(nchunk):
        nTi = nT[i % 2]
        knT = key("nT", i % 2)
        qa = q_aug[i % 2]
        kqa = key("qaug", i % 2)
        c0 = i * 512
        for tl in range(4):
            tt = i * 4 + tl
            b = tt % 2
            x_t, s_t, n_t = xt[b], st[b], nb[b]
            kx, ks, kn = key("xt", b), key("st", b), key("nb")
            P.dma("sync", x_t[:], (d["xb_tile"](tt) if "xb_tile" in d else d["xb"][tt * 128:(tt + 1) * 128, :]), writes=[kx])
            P.op(SC, lambda e, x_t=x_t, s_t=s_t, n_t=n_t: e.activation(out=n_t[:], in_=x_t[:], func=AF.Square,
                                                              accum_out=s_t[:, 0:1]),
                 reads=[kx], writes=[kn, ks])
            P.op(SC, lambda e, s_t=s_t: e.activation(out=s_t[:, 1:2], in_=s_t[:, 0:1], func=AF.Ln, scale=1.0 / 1024,
                                                     bias=eps[:]),
                 reads=[ks, key("eps")], writes=[ks])
            P.op(SC, lambda e, s_t=s_t: e.activation(out=s_t[:, 2:3], in_=s_t[:, 1:2], func=AF.Exp, scale=-0.5),
                 reads=[ks], writes=[ks])
            P.op(V, lambda e, x_t=x_t, s_t=s_t, n_t=n_t: e.scalar_tensor_tensor(
                out=n_t[:], in0=x_t[:], scalar=s_t[:, 2:3], in1=gmix[:], op0=ALU.mult, op1=ALU.mult),
                reads=[kx, ks, key("gmix")], writes=[kn])
            for kc in range(8):
                P.op(T, lambda e, kc=kc, n_t=n_t: e.transpose(out=pT[:, kc, :], in_=n_t[:, kc * 128:(kc + 1) * 128],
                                                              identity=ident_b[:]),
                     reads=[kn, key("ident_b")], writes=[key("pT")])
            P.op(V, lambda e, tl=tl, nTi=nTi: e.tensor_copy(out=nTi[:, :, tl * 128:(tl + 1) * 128], in_=pT[:]),
                 reads=[key("pT")], writes=[knT + (tl,)])

        def proj(ps, kps, col0, ncol, npart_out=None):
            for kc in range(8):
                P.op(T, lambda e, kc=kc: e.matmul(ps[0:ncol, :], lhsT=wb[:, kc, col0:col0 + ncol], rhs=nTi[:, kc, :],
                                                  start=(kc == 0), stop=(kc == 7)),
                     reads=[key("wb"), knT], writes=[kps])

        if stop <= 1:
            continue
        st_ = {}

        def S1(c):
            zx_ps, kzx = next_pa()
            proj(zx_ps, kzx, c * 128, 128)
            P.op(SC, lambda e: e.copy(out=zxb[c][:, 3:515], in_=zx_ps[:]),
                 reads=[kzx], writes=[key("zxb", c, "main")])

        def S2(c):
            P.op(V, lambda e: e.tensor_scalar(out=xc[c][:], in0=zxb[c][:, 3:515], scalar1=pvc(3 * 2 + c),
                                              scalar2=pvc(8 + c), op0=ALU.mult, op1=ALU.add),
                 reads=[key("zxb", c), key("pv")], writes=[key("xc", c)])
            for dl in (1, 2, 3):
                P.op(V, lambda e, dl=dl: e.scalar_tensor_tensor(
                    out=xc[c][:], in0=zxb[c][:, 3 - dl:515 - dl], scalar=pvc((3 - dl) * 2 + c), in1=xc[c][:],
                    op0=ALU.mult, op1=ALU.add),
                    reads=[key("zxb", c), key("pv"), key("xc", c)], writes=[key("xc", c)])
            P.op(G, lambda e: e.tensor_copy(out=zxb[c][:, 0:3], in_=zxb[c][:, 512:515]),
                 reads=[key("zxb", c, "main")], writes=[key("zxb", c, "halo")])
            P.op(G, lambda e: e.tensor_copy(out=xcb[c][:], in_=xc[c][:]), reads=[key("xc", c)], writes=[key("xcb", c)])

        def S3(c):
            r_ps, kr = next_pa()
            P.op(T, lambda e: e.matmul(r_ps[:], lhsT=wr[:, c, :], rhs=xcb[c][:], start=True, stop=True),
                 reads=[key("wr"), key("xcb", c)], writes=[kr])
            i_ps, ki = next_pa()
            P.op(T, lambda e: e.matmul(i_ps[:], lhsT=wi[:, c, :], rhs=xcb[c][:], start=True, stop=True),
                 reads=[key("wi"), key("xcb", c)], writes=[ki])
            st_[c] = (r_ps, kr, i_ps, ki)

        def S4(c):
            r_ps, kr, i_ps, ki = st_[c]
            P.op(SC, lambda e: e.activation(out=rr[c][:], in_=r_ps[:], func=AF.Sigmoid, bias=pvc(10 + c)),
                 reads=[kr, key("pv")], writes=[key("rr", c)])
            P.op(SC, lambda e: e.activation(out=ig[c][:], in_=i_ps[:], func=AF.Sigmoid, bias=pvc(12 + c)),
                 reads=[ki, key("pv")], writes=[key("ig", c)])

        def S5(c):
            P.op(SC, lambda e: e.activation(out=aa[c][:], in_=rr[c][:], func=AF.Exp, scale=cl[:, c, 0:1]),
                 reads=[key("rr", c), key("cl")], writes=[key("aa", c)])
            P.op(SC, lambda e: e.activation(out=a2[c][:], in_=rr[c][:], func=AF.Exp, scale=cl[:, c, 1:2]),
                 reads=[key("rr", c), key("cl")], writes=[key("a2", c)])
            P.op(G, lambda e: e.tensor_tensor(out=uu[c][:], in0=ig[c][:], in1=xc[c][:], op=ALU.mult),
                 reads=[key("ig", c), key("xc", c)], writes=[key("uu", c)])

        def S5b(c):
            P.op(SC, lambda e: e.activation(out=sq_[c][:], in_=a2[c][:], func=AF.Sqrt, scale=-1.0, bias=one1[:]),
                 reads=[key("a2", c), key("one1")], writes=[key("sq_", c)])

        def S6(c):
            P.op(V, lambda e: e.tensor_tensor(out=uu[c][:], in0=uu[c][:], in1=sq_[c][:], op=ALU.mult),
                 reads=[key("uu", c), key("sq_", c)], writes=[key("uu", c)])
            P.op(V, lambda e: e.tensor_tensor_scan(out=hh[c][:], data0=aa[c][:], data1=uu[c][:],
                                                   initial=hst[:, c:c + 1], op0=ALU.mult, op1=ALU.add),
                 reads=[key("aa", c), key("uu", c), key("hst", c)], writes=[key("hh", c)])
            P.op(G, lambda e: e.tensor_copy(out=hst[:, c:c + 1], in_=hh[c][:, 511:512]),
                 reads=[key("hh", c)], writes=[key("hst", c)])

        def S7(c):
            zg_ps, kzg = next_pa()
            proj(zg_ps, kzg, 256 + c * 128, 128)
            P.op(SC, lambda e: e.activation(out=gg[c][:], in_=zg_ps[:], func=AF.Gelu_apprx_tanh),
                 reads=[kzg], writes=[key("gg", c)])

        def S8(c):
            P.op(V, lambda e: e.tensor_tensor(out=yy[c][:], in0=hh[c][:], in1=gg[c][:], op=ALU.mult),
                 reads=[key("hh", c), key("gg", c)], writes=[key("yy", c)])
            P.op(G, lambda e: e.tensor_tensor(out=ysq[c][:], in0=yy[c][:], in1=yy[c][:], op=ALU.mult),
                 reads=[key("yy", c)], writes=[key("ysq", c)])
            P.op(SC, lambda e: e.activation(out=ygb[c][:], in_=yy[c][:], func=AF.Identity, scale=pvc(16 + c)),
                 reads=[key("yy", c), key("pv")], writes=[key("ygb", c)])
            P.dma("sync", d["yT"][c * 128:(c + 1) * 128, c0:c0 + 512], ygb[c][:], reads=[key("ygb", c)],
                  writes=[key("yT_out")])

        for stg in (S1, S2, S3, S7, S4, S5, S5b, S6, S8):
            for c in range(2):
                stg(c)

        if stop <= 2:
            continue
        pairs = [(which, hp) for which in range(2) for hp in range(2)]
        zs = {}

        def qk_proj(n):
            which, hp = pairs[n]
            z_ps, kz = next_pa()
            proj(z_ps, kz, 512 + which * 256 + hp * 128, 128)
            zs[n] = (z_ps, kz)

        def qk_norm(n):
            which, hp = pairs[n]
            z_ps, kz = zs[n]
            par = n % 2
            P.op(SC, lambda e: e.activation(out=qsq[par][:], in_=z_ps[:], func=AF.Square),
                 reads=[kz], writes=[key("qsq", par)])
            s_ps, ks_ = next_pa()
            P.op(T, lambda e: e.matmul(s_ps[:], lhsT=ones_bd[:], rhs=qsq[par][:], start=True, stop=True),
                 reads=[key("ones_bd"), key("qsq", par)], writes=[ks_])
            P.op(SC, lambda e: e.activation(out=qlr[par][:], in_=s_ps[:], func=AF.Ln, scale=1.0 / 64, bias=eps[:]),
                 reads=[ks_, key("eps")], writes=[key("qlr", par)])
            P.op(SC, lambda e: e.activation(out=qrs[par][:], in_=qlr[par][:], func=AF.Exp, scale=-0.5),
                 reads=[key("qlr", par)], writes=[key("qrs", par)])
            gcol = gq8[:, 0:1] if which == 0 else pv[:, 19:20]
            kg = key("gq8") if which == 0 else key("pv")
            tmp = Pt[n % 4]
            ktmp = key("Pt", n % 4)
            hlA, hlB = 2 * hp, 2 * hp + 1
            if which == 0:
                dstA, kdA = qa[0:64, hlA, :], kqa + ("q", hlA)
                dstB, kdB = qa[0:64, hlB, :], kqa + ("q", hlB)
            else:
                dstA, kdA = k_aug[0:64, hlA, c0:c0 + 512], key("kaug", "k", hlA, i)
                dstB, kdB = k_aug[0:64, hlB, c0:c0 + 512], key("kaug", "k", hlB, i)
            P.op(V, lambda e: e.scalar_tensor_tensor(
                out=dstA, in0=qrs[par][0:64, :], scalar=gcol[0:64, :], in1=z_ps[0:64, :], op0=ALU.mult, op1=ALU.mult),
                reads=[key("qrs", par), kg, kz], writes=[kdA])
            P.op(V, lambda e: e.scalar_tensor_tensor(
                out=tmp[64:128, :], in0=qrs[par][64:128, :], scalar=gcol[64:128, :], in1=z_ps[64:128, :],
                op0=ALU.mult, op1=ALU.mult),
                reads=[key("qrs", par), kg, kz], writes=[ktmp])
            P.dma("sync", dstB, tmp[64:128, :], reads=[ktmp], writes=[kdB])

        qk_proj(0)
        for n in range(4):
            if n + 1 < 4:
                qk_proj(n + 1)
            qk_norm(n)

        for tl in range(4):
            j = i * 4 + tl
            v_ps, kv = next_pa()
            for kc in range(8):
                P.op(T, lambda e, kc=kc, tl=tl, v_ps=v_ps: e.matmul(
                    v_ps[:, 0:256], lhsT=nTi[:, kc, tl * 128:(tl + 1) * 128], rhs=wb[:, kc, 1024:1280],
                    start=(kc == 0), stop=(kc == 7)),
                    reads=[key("wb"), knT], writes=[kv])
            P.op(SC, lambda e, j=j, v_ps=v_ps: e.copy(out=Vp[:, j, :, 0:64],
                                                      in_=v_ps[:, 0:256].rearrange("p (h d) -> p h d", h=4)),
                 reads=[kv], writes=[key("Vp", "v", j)])
            for kc in range(8):
                P.op(T, lambda e, kc=kc, tl=tl: e.matmul(
                    misc[:, 0:4], lhsT=nTi[:, kc, tl * 128:(tl + 1) * 128], rhs=wb[:, kc, 1280:1284],
                    start=(kc == 0), stop=(kc == 7)),
                    reads=[key("wb"), knT], writes=[key("misc")])
            P.op(V, lambda e: e.tensor_tensor(out=ft[:], in0=misc[:, 0:4], in1=bfb[:], op=ALU.add),
                 reads=[key("misc"), key("bfb")], writes=[key("ft")])
            P.op(SC, lambda e: e.activation(out=fe[:], in_=ft[:], func=AF.Exp, scale=-1.0),
                 reads=[key("ft")], writes=[key("fe")])
            P.op(SC, lambda e: e.activation(out=nl, in_=fe[:], func=AF.Ln, bias=one1[:]),
                 reads=[key("fe"), key("one1")], writes=[key("nl")])
            if stop <= 3.4:
                continue
            P.op(T, lambda e: e.matmul(misc[:, 8:12], lhsT=tri_f[:], rhs=nl, start=True, stop=True),
                 reads=[key("tri_f"), key("nl")], writes=[key("misc")])
            P.op(T, lambda e: e.matmul(misc[:, 16:20], lhsT=ones_f[:], rhs=nl, start=True, stop=True),
                 reads=[key("ones_f"), key("nl")], writes=[key("misc")])
            P.op(V, lambda e, j=j: e.tensor_tensor(out=nc_tok[:, j, :], in0=misc[:, 8:12], in1=carry[:], op=ALU.add),
                 reads=[key("misc"), key("carry")], writes=[key("nc_tok", j)])
            P.op(V, lambda e: e.tensor_tensor(out=carry[:], in0=misc[:, 16:20], in1=carry[:], op=ALU.add),
                 reads=[key("misc"), key("carry")], writes=[key("carry")])
            P.op(V, lambda e, tl=tl: e.tensor_copy(out=nlb[:, tl, :, 64], in_=nl), reads=[key("nl")], writes=[key("nlb", tl)])
        cr = cref[i % 2]
        kcr = key("cref", i % 2)
        P.op(V, lambda e, cr=cr: e.tensor_copy(out=cr[:], in_=carry[:]), reads=[key("carry")], writes=[kcr])
        for hl in range(4):
            for a_ in range(4):
                for a2_ in range(a_, 4):
                    P.op(T, lambda e, hl=hl, a_=a_, a2_=a2_: e.matmul(
                        misc[0:65, 384:512],
                        lhsT=nlb[:, a2_, hl, :], rhs=(low_b[:] if a2_ == a_ else ones_b[:]),
                        start=(a2_ == a_), stop=(a2_ == 3)),
                        reads=[key("nlb"), key("low_b"), key("ones_b")], writes=[key("misc")])
                P.op(V, lambda e, hl=hl, a_=a_: e.tensor_copy(out=qa[64:65, hl, a_ * 128:(a_ + 1) * 128],
                                                             in_=misc[64:65, 384:512]),
                     reads=[key("misc")], writes=[kqa + ("aug", hl, a_)])
        if stop <= 3.9:
            continue
        bt = bias_t[i % 2]
        kbt = key("bias", i % 2)
        nj = 4 * i + 4
        for hl in range(4):
            P.op(V, lambda e, hl=hl, bt=bt, cr=cr, nj=nj: e.tensor_scalar(
                out=bt[:, 0:nj, hl], in0=nc_tok[:, 0:nj, hl], scalar1=cr[:, hl:hl + 1], scalar2=None,
                op0=ALU.subtract),
                reads=[key("nc_tok"), kcr], writes=[kbt])

        if stop <= 4:
            continue
        for hl in range(4):
            def emit_S(j, hl=hl):
                m = j - 4 * i
                off = 128 * m if m > 0 else 0
                sp, ksp = s_pool[j % 4]
                pt = Pt[j % 4]
                kpt = key("Pt", j % 4)
                P.op(T, lambda e: e.matmul(
                    sp[:, off:512], lhsT=k_aug[0:65, hl, j * 128:(j + 1) * 128], rhs=qa[0:65, hl, off:512],
                    start=True, stop=True),
                    reads=[key("kaug"), kqa], writes=[ksp])
                P.op(SC, lambda e: e.activation(
                    out=pt[:, off:512], in_=sp[:, off:512], func=AF.Exp, bias=bt[:, j, hl:hl + 1]),
                    reads=[ksp, kbt], writes=[kpt])
                if m >= 0:
                    P.op(G, lambda e: e.tensor_tensor(out=pt[:, off:off + 128], in0=pt[:, off:off + 128],
                                                      in1=tri_b[:], op=ALU.mult),
                         reads=[kpt, key("tri_b")], writes=[kpt])

            def emit_O(j, hl=hl):
                m = j - 4 * i
                off = 128 * m if m > 0 else 0
                pt = Pt[j % 4]
                kpt = key("Pt", j % 4)
                P.op(T, lambda e: e.matmul(
                    ops_[:, off:512], lhsT=Vp[:, j, hl, :], rhs=pt[:, off:512], start=(j == 0), stop=(j == nj - 1)),
                    reads=[key("Vp"), kpt], writes=[key("ops")])

            for j0 in range(min(3, nj)):
                emit_S(j0)
            for j in range(nj):
                if j + 3 < nj:
                    emit_S(j + 3)
                emit_O(j)
            P.op(SC, lambda e: e.copy(out=den_sb[64:128, :], in_=ops_[64:128, :]), reads=[key("ops")],
                 writes=[key("den_sb")])
            P.op(V, lambda e: e.tensor_copy(out=dhi[64:128, :], in_=den_sb[64:128, :]), reads=[key("den_sb")],
                 writes=[key("dhi")])
            P.op(V, lambda e: e.tensor_tensor(out=dlo[64:128, :], in0=den_sb[64:128, :], in1=dhi[64:128, :],
                                              op=ALU.subtract),
                 reads=[key("den_sb"), key("dhi")], writes=[key("dlo")])
            P.op(T, lambda e: e.matmul(dps[0:64, :], lhsT=shb[:], rhs=dhi[:], start=True, stop=False),
                 reads=[key("shb"), key("dhi")], writes=[key("dps")])
            P.op(T, lambda e: e.matmul(dps[0:64, :], lhsT=shb[:], rhs=dlo[:], start=False, stop=True),
                 reads=[key("shb"), key("dlo")], writes=[key("dps")])
            lden, rinv = qlr[hl % 2][0:64, :], qrs[hl % 2][0:64, :]
            P.op(SC, lambda e: e.activation(out=lden, in_=dps[0:64, :], func=AF.Ln),
                 reads=[key("dps")], writes=[key("qlr", hl % 2)])
            P.op(SC, lambda e: e.activation(out=rinv, in_=lden, func=AF.Exp, scale=-1.0),
                 reads=[key("qlr", hl % 2)], writes=[key("qrs", hl % 2)])
            P.op(V, lambda e: e.tensor_tensor(out=yat[:], in0=ops_[0:64, :], in1=rinv, op=ALU.mult),
                 reads=[key("ops"), key("qrs", hl % 2)], writes=[key("yat")])
            P.op(G, lambda e, hl=hl: e.tensor_tensor(out=asq[hl][:], in0=yat[:], in1=yat[:], op=ALU.mult),
                 reads=[key("yat")], writes=[key("asq", hl)])
            P.op(SC, lambda e, hl=hl: e.activation(out=agb[hl % 2][:], in_=yat[:], func=AF.Copy,
                                                   scale=pv[0:64, 20 + hl:21 + hl]),
                 reads=[key("yat"), key("pv")], writes=[key("agb", hl % 2)])
            P.dma("sync", d["yT"][256 + hl * 64:256 + (hl + 1) * 64, c0:c0 + 512], agb[hl % 2][:],
                  reads=[key("agb", hl % 2)], writes=[key("yT_out")])
        if stop <= 5:
            continue
        for tl in range(4):
            for c in range(2):
                P.op(T, lambda e, tl=tl, c=c: e.matmul(misc[:, 256 + tl * 2:257 + tl * 2],
                                                       lhsT=ysq[c][:, tl * 128:(tl + 1) * 128], rhs=ones_b[:, 0:1],
                                                       start=(c == 0), stop=(c == 1)),
                     reads=[key("ysq", c), key("ones_b")], writes=[key("misc")])
            for hl in range(4):
                P.op(T, lambda e, tl=tl, hl=hl: e.matmul(misc[:, 257 + tl * 2:258 + tl * 2],
                                                         lhsT=asq[hl][:, tl * 128:(tl + 1) * 128],
                                                         rhs=ones_b[0:64, 0:1], start=(hl == 0), stop=(hl == 3)),
                     reads=[key("asq", hl), key("ones_b")], writes=[key("misc")])
        P.op(V, lambda e: e.tensor_copy(out=sso[:], in_=misc[:, 256:264].rearrange("p (t k) -> p t k", k=2)),
             reads=[key("misc")], writes=[key("sso")])
        P.dma("sync", d["ss"][c0:c0 + 512, :].rearrange("(t p) k -> p t k", p=128), sso[:], reads=[key("sso")],
              writes=[key("ss_out")])
    return [key("yT_out"), key("ss_out")]


CAP = 512
NSLOT = 32 * CAP
TABW = 16


def token_phase(P, nc, d, NT=2048, pfx="t", nexp=32, gather=False, after_tile=None):
    def key(*a):
        return (pfx + "_" + str(a[0]),) + tuple(a[1:])

    sb = lambda shape, dt, name: P.sb(shape, dt, pfx + "_" + name)
    NTL = NT // 128
    NCH = NT // 512
    V, SC, G, T = "vector", "scalar", "gpsimd", "tensor"

    xres = sb([128, NTL, 1024], F32, "xres")
    nTt = sb([128, 8, 128], BF16, "nTt")
    XT1 = sb([128, 8, 512], BF16, "XT")
    XT = [XT1, XT1]
    Xg = [sb([128, 4, 1024], BF16, f"Xg{i}") for i in range(2)]
    ysb = [sb([128, 1024], BF16, f"ysb{i}") for i in range(2)]
    idxe = [sb([128, 4, TABW], I32, f"idxe{i}") for i in range(2)]
    zt = sb([128, 16, TABW], I32, "zt")
    tidt = sb([128, NTL, TABW], I32, "tidt")
    posi = sb([128, NTL, 2], I32, "posi")
    w12 = sb([128, NTL, 2], F32, "w12")
    cnt = sb([128, 32], F32, "cnt")
    eci = sb([128, 32], I32, "eci")
    ec = sb([128, 32], F32, "ec")
    dci = sb([128, 1], I32, "dci")
    dcol = sb([128, 1], F32, "dcol")
    Aoh = sb([128, 32], F32, "Aoh")
    Ab = sb([128, 32], BF16, "Ab")
    wfull = sb([128, 32], F32, "wfull")
    rank = sb([128, 32], F32, "rank")
    val = sb([128, 32], F32, "val")
    tmp32 = sb([128, 32], F32, "tmp32")
    m8v = sb([128, 8], F32, "m8v")
    posf = sb([128, 2], F32, "posf")
    negf = sb([128, 2], F32, "negf")
    triS = sb([128, 128], BF16, "triS")
    eg = [sb([128, 8, 512], BF16, f"eg{i}") for i in range(2)]
    eu = [sb([128, 8, 512], BF16, f"eu{i}") for i in range(2)]
    ed = [sb([128, 4, 1024], BF16, f"ed{i}") for i in range(2)]
    wbig = sb([128, 8, 1024], BF16, "wbig")
    wpp = sb([128, 2, 1024], BF16, "wpp")
    hT = sb([128, 4, 512], BF16, "hT")
    sgt = [sb([128, 512], F32, f"sgt{i}") for i in range(2)]
    bc1 = sb([128, 1024], F32, "bc1")
    bc2 = sb([128, 1024], F32, "bc2")
    w1 = sb([128, 1024], F32, "w1")
    w2 = sb([128, 1024], F32, "w2")
    nhi = sb([128, 1024], BF16, "nhi")
    nlo = sb([128, 1024], BF16, "nlo")
    nTlo = sb([128, 8, 128], BF16, "nTlo")
    yts = [sb([128, 2, 4, 128], BF16, f"yt{i}") for i in range(2)]
    pt_ = sb([128, 256], F32, "pt")
    pb = sb([128, 256], BF16, "pb")
    pTs = sb([128, 2, 128], BF16, "pTs")
    wrf = sb([128, 8, 36], F32, "wrf")
    wrh = sb([128, 8, 36], BF16, "wrh")
    wrl = sb([128, 8, 36], BF16, "wrl")
    brt = sb([128, 36], F32, "brt")
    ident_b = sb([128, 128], BF16, "ident_b")
    ones_b = sb([128, 128], BF16, "ones_b")
    eps = sb([128, 1], F32, "eps")
    ssts = [sb([128, 2, 2], F32, f"sst{i}") for i in range(4)]
    sm = sb([128, 16], F32, "sm")
    lgs = sb([128, 36], F32, "lgs")
    oh = sb([128, 4], F32, "oh")
    ge = sb([128, 4], F32, "ge")
    el8 = sb([128, 8], F32, "el8")
    m8 = sb([128, 8], F32, "m8")
    sel8 = sb([128, 8], F32, "sel8")
    ee8 = sb([128, 8], F32, "ee8")
    w8 = sb([128, 8], F32, "w8")
    A = [P.ps([128, 512], F32, pfx + f"_A{i}") for i in range(2)]
    B = [P.ps([128, 512], F32, pfx + f"_B{i}") for i in range(2)]
    Y = [P.ps([128, 512], F32, pfx + f"_Y{i}") for i in range(2)]
    pT = P.ps([128, 8, 128], BF16, pfx + "_pT")
    misc = P.ps([128, 512], F32, pfx + "_misc")

    if gather:
        idxy = sb([128, 32], I32, "idxy")
        idxs = sb([128, 2 * NTL], I32, "idxs")
        P.dma("sync", idxy[:], d["idx_y4"][:, :], writes=[key("idxy")])
        P.dma("sync", idxs[:], d["idx_s"][:, :], writes=[key("idxs")])
    P.dma("sync", bc1[:], d["ffn_norm"].partition_broadcast(128), writes=[key("bc1")])
    P.dma("sync", brt[:], d["brt"].partition_broadcast(128), writes=[key("brt")])
    P.dma("sync", wrf[:], d["wrt"].rearrange("(c p) n -> p c n", p=128), writes=[key("wrf")])
    P.dma("gpsimd", wbig[:], d["wout"].rearrange("(c p) n -> p c n", p=128), writes=[key("wbig")])
    P.dma("gpsimd", wpp[:], d["wpp"].rearrange("(c p) n -> p c n", p=128), writes=[key("wpp")])
    P.op(V, lambda e: e.memset(eps[:], EPS), writes=[key("eps")])
    P.op(V, lambda e: e.memset(ones_b[:], 1.0), writes=[key("ones_b")])
    P.op(G, lambda e: e.affine_select(out=ident_b[:], in_=ones_b[:], pattern=[[1, 128]], compare_op=ALU.is_equal,
                                      fill=0.0, base=0, channel_multiplier=-1),
         reads=[key("ones_b")], writes=[key("ident_b")])
    P.op(G, lambda e: e.affine_select(out=triS[:], in_=ones_b[:], pattern=[[1, 128]], compare_op=ALU.is_gt,
                                      fill=0.0, base=0, channel_multiplier=-1),
         reads=[key("ones_b")], writes=[key("triS")])
    P.op(G, lambda e: e.iota(tidt[:], pattern=[[128, NTL], [0, TABW]], base=0, channel_multiplier=1),
         writes=[key("tidt")])
    P.op(G, lambda e: e.iota(eci[:], pattern=[[CAP, 32]], base=1, channel_multiplier=0), writes=[key("eci")])
    P.op(G, lambda e: e.iota(dci[:], pattern=[[0, 1]], base=NSLOT + 1, channel_multiplier=1), writes=[key("dci")])
    P.op(V, lambda e: e.tensor_copy(out=ec[:], in_=eci[:]), reads=[key("eci")], writes=[key("ec")])
    P.op(V, lambda e: e.tensor_copy(out=dcol[:], in_=dci[:]), reads=[key("dci")], writes=[key("dcol")])
    P.op(V, lambda e: e.memset(cnt[:], 0.0), writes=[key("cnt")])
    P.op(V, lambda e: e.memset(zt[:], 0), writes=[key("zt")])
    P.op(V, lambda e: e.memset(nlo[:], 0.0), writes=[key("nlo")])
    tabv = d["tab"].rearrange("(p j) w -> p j w", p=128)
    for j8 in range(8):
        P.dma("sync", tabv[:, j8 * 16:(j8 + 1) * 16, :], zt[:], reads=[key("zt")], writes=[("tab", "z", j8)])
    P.dma("sync", tabv[:, 128:129, :], zt[:, 0:1, :], reads=[key("zt")], writes=[("tab", "z", 8)])
    P.dma("sync", d["ytab"][NSLOT:NSLOT + 128, :], nlo[:], reads=[key("nlo")], writes=[("ytab", "dump")])
    P.op(V, lambda e: e.tensor_copy(out=wrh[:], in_=wrf[:]), reads=[key("wrf")], writes=[key("wrh")])
    P.op(V, lambda e: e.tensor_tensor(out=wrl[:], in0=wrf[:], in1=wrh[:], op=ALU.subtract),
         reads=[key("wrf"), key("wrh")], writes=[key("wrl")])

    def load_expert(e_):
        b = e_ % 2
        P.dma("gpsimd", eg[b][:], d["weg"][e_].rearrange("(c p) n -> p c n", p=128), writes=[key("eg", b)])
        P.dma("gpsimd", eu[b][:], d["weu"][e_].rearrange("(c p) n -> p c n", p=128), writes=[key("eu", b)])
        P.dma("gpsimd", ed[b][:], d["wed"][e_].rearrange("(c p) n -> p c n", p=128), writes=[key("ed", b)])

    def rms(x_ap, kx, gt, kg, out_f32=None):
        P.op(SC, lambda e: e.activation(out=nlo[:], in_=x_ap, func=AF.Square, accum_out=sm[:, 0:1]),
             reads=[kx], writes=[key("nlo"), key("sm", 0)])
        P.op(SC, lambda e: e.activation(out=sm[:, 1:2], in_=sm[:, 0:1], func=AF.Ln, scale=1.0 / 1024, bias=eps[:]),
             reads=[key("sm", 0), key("eps")], writes=[key("sm", 1)])
        P.op(SC, lambda e: e.activation(out=sm[:, 2:3], in_=sm[:, 1:2], func=AF.Exp, scale=-0.5),
             reads=[key("sm", 1)], writes=[key("sm", 2)])
        P.op(V, lambda e: e.scalar_tensor_tensor(out=w1[:], in0=x_ap, scalar=sm[:, 2:3], in1=gt[:], op0=ALU.mult,
                                                 op1=ALU.mult),
             reads=[kx, key("sm", 2), kg], writes=[key("w1")])

    def transpose8(src, ksrc, dst_ap, kdst):
        for kc in range(8):
            P.op(T, lambda e, kc=kc: e.transpose(out=pT[:, kc, :], in_=src[:, kc * 128:(kc + 1) * 128],
                                                 identity=ident_b[:]),
                 reads=[ksrc, key("ident_b")], writes=[key("pT")])
        P.op(V, lambda e: e.tensor_copy(out=dst_ap, in_=pT[:]), reads=[key("pT")], writes=[kdst])

    def loads(tt):
        t0 = tt * 128
        yt = yts[tt % 2]
        sst = ssts[tt % 4]
        kyt = key("yt%d" % (tt % 2))
        ksst = key("sst%d" % (tt % 4))
        P.dma("sync", xres[:, tt, :], d["x"][t0:t0 + 128, :], writes=[key("xres", tt)])
        if gather:
            for g in range(2):
                cols = g * NTL + tt
                P.op(G, lambda e, g=g, cols=cols: e.indirect_dma_start(
                    out=sst[:, g, :], out_offset=None, in_=d["ssg"][:, :],
                    in_offset=bass.IndirectOffsetOnAxis(ap=idxs[:, cols:cols + 1], axis=0)),
                    reads=[key("idxs")], writes=[ksst + (g,)], dma=True)
        else:
            for g in range(2):
                P.dma("sync", yt[:, g, :, :], d["yT"][g, :, t0:t0 + 128].rearrange("(c p) t -> p c t", p=128),
                      writes=[kyt + (g,)])
            P.dma("sync", sst[:], d["ss"][:, t0:t0 + 128, :].rearrange("g p k -> p g k"), writes=[ksst])

    def ytg_view(grp):
        return Xg[grp % 2][:].rearrange("p s (a t) -> p (s a) t", a=2)

    def group_gathers(grp):
        v = ytg_view(grp)
        for q in range(8):
            col = q * 4 + grp
            P.op(G, lambda e, q=q, col=col: e.indirect_dma_start(
                out=v[:, q, :], out_offset=None, in_=d["yTg4"][:, :],
                in_offset=bass.IndirectOffsetOnAxis(ap=idxy[:, col:col + 1], axis=0)),
                reads=[key("idxy")], writes=[key("Xg", grp % 2, q // 2, q % 2)], dma=True)

    load_expert(0)
    if gather:
        group_gathers(0)

    okey = key
    SCR = ("sm", "w1", "nhi", "nlo", "nTt", "nTlo", "lgs", "ge", "oh", "el8", "m8", "sel8", "ee8", "w8", "wfull",
           "Aoh", "Ab", "rank", "tmp32", "val", "m8v", "posf", "negf")
    small = dict(lgs=([128, 36], F32), ge=([128, 4], F32), oh=([128, 4], F32), el8=([128, 8], F32), m8=([128, 8], F32),
                 sel8=([128, 8], F32), ee8=([128, 8], F32), w8=([128, 8], F32), wfull=([128, 32], F32),
                 Aoh=([128, 32], F32), Ab=([128, 32], BF16), rank=([128, 32], F32), tmp32=([128, 32], F32),
                 val=([128, 32], F32), m8v=([128, 8], F32), posf=([128, 2], F32), negf=([128, 2], F32),
                 sm=([128, 16], F32))
    hTb = hT[:].rearrange("p a b -> p (a b)")
    t1s = [dict(sm=sm, w1=w1, nhi=nhi, nlo=nlo, nTt=nTt, nTlo=nTlo, lgs=lgs, ge=ge, oh=oh, el8=el8, m8=m8, sel8=sel8,
                ee8=ee8, w8=w8, wfull=wfull, Aoh=Aoh, Ab=Ab, rank=rank, tmp32=tmp32, val=val, m8v=m8v, posf=posf,
                negf=negf),
           dict(w1=XT1[:].rearrange("p k t -> p (k t)").bitcast(F32)[:, 0:1024], nhi=ysb[0][:], nlo=ysb[1][:],
                nTt=hTb[:, 0:1024].rearrange("p (k t) -> p k t", k=8),
                nTlo=hTb[:, 1024:2048].rearrange("p (k t) -> p k t", k=8))]
    for nm_, (shp_, dt_) in small.items():
        t1s[1][nm_] = sb(shp_, dt_, nm_ + "_b")

    def t1_tile(tt):
        t0 = tt * 128
        par = tt % 2
        TT = t1s[par]
        sm, w1, nhi, nlo, nTt, nTlo = TT["sm"], TT["w1"], TT["nhi"], TT["nlo"], TT["nTt"], TT["nTlo"]
        lgs, ge, oh, el8, m8, sel8, ee8, w8 = (TT[n] for n in ("lgs", "ge", "oh", "el8", "m8", "sel8", "ee8", "w8"))
        wfull, Aoh, Ab, rank, tmp32, val, m8v, posf, negf = (TT[n] for n in ("wfull", "Aoh", "Ab", "rank", "tmp32",
                                                                               "val", "m8v", "posf", "negf"))
        PSs = [(A, "A"), (B, "B")] if par == 0 else [(Y, "Y"), (Y, "Y")]

        def key(*a):
            if par == 0 or a[0] not in SCR:
                return okey(*a)
            big = {"w1": ("XT",), "nhi": ("ysb", 0), "nlo": ("ysb", 1), "nTt": ("hT",), "nTlo": ("hT",)}
            if a[0] in big:
                return okey(*big[a[0]])
            return okey(a[0] + "_b", *a[1:])

        def transpose8(src, ksrc, dst_ap, kdst):
            P.hold()
            for kc in range(8):
                P.op(T, lambda e, kc=kc: e.transpose(out=pT[:, kc, :], in_=src[:, kc * 128:(kc + 1) * 128],
                                                     identity=ident_b[:]),
                     reads=[ksrc, key("ident_b")], writes=[key("pT")])
            P.op(V, lambda e: e.tensor_copy(out=dst_ap, in_=pT[:]), reads=[key("pT")], writes=[kdst])
            P.release()
        t0 = tt * 128
        kxr = key("xres", tt)
        yt = yts[tt % 2]
        sst = ssts[tt % 4]
        kyt = key("yt%d" % (tt % 2))
        ksst = key("sst%d" % (tt % 4))
        P.op(V, lambda e: e.tensor_tensor(out=sm[:, 4:6], in0=sst[:, 0, :], in1=sst[:, 1, :], op=ALU.add),
             reads=[ksst], writes=[key("sm", 4)])
        P.op(SC, lambda e: e.activation(out=sm[:, 6:8], in_=sm[:, 4:6], func=AF.Ln, scale=1.0 / 512, bias=eps[:]),
             reads=[key("sm", 4), key("eps")], writes=[key("sm", 6)])
        P.op(SC, lambda e: e.activation(out=sm[:, 8:10], in_=sm[:, 6:8], func=AF.Exp, scale=-0.5),
             reads=[key("sm", 6)], writes=[key("sm", 8)])
        for grp in range(2):
            PS, kPS = PSs[grp]
            for half in range(2):
                n_ = 0
                for g in range(2):
                    for c in range(2):
                        row0 = grp * 512 + g * 256 + c * 128
                        kc = row0 // 128
                        if gather:
                            q_ = g * 4 + grp * 2 + c
                            lhs = ytg_view(tt // 4)[:, q_, (tt % 4) * 128:(tt % 4 + 1) * 128]
                            klhs = okey("Xg", (tt // 4) % 2, q_ // 2, q_ % 2)
                        else:
                            lhs = yt[:, g, grp * 2 + c, :]
                            klhs = kyt + (g,)
                        P.op(T, lambda e, lhs=lhs, kc=kc, half=half, PS=PS, n_=n_: e.matmul(
                            PS[half][:], lhsT=lhs, rhs=wbig[:, kc, half * 512:(half + 1) * 512],
                            start=(n_ == 0), stop=(n_ == 3)),
                            reads=[klhs, okey("wbig")], writes=[okey(kPS, half)])
                        n_ += 1
            for half in range(2):
                P.op(V, lambda e, grp=grp, half=half, PS=PS: e.scalar_tensor_tensor(
                    out=xres[:, tt, half * 512:(half + 1) * 512], in0=PS[half][:], scalar=sm[:, 8 + grp:9 + grp],
                    in1=xres[:, tt, half * 512:(half + 1) * 512], op0=ALU.mult, op1=ALU.add),
                    reads=[okey(kPS, half), key("sm", 8), kxr], writes=[kxr])
        P.op(SC, lambda e: e.activation(out=nlo[:], in_=xres[:, tt, :], func=AF.Square, accum_out=sm[:, 0:1]),
             reads=[kxr], writes=[key("nlo"), key("sm", 0)])
        P.op(SC, lambda e: e.activation(out=sm[:, 1:2], in_=sm[:, 0:1], func=AF.Ln, scale=1.0 / 1024, bias=eps[:]),
             reads=[key("sm", 0), key("eps")], writes=[key("sm", 1)])
        P.op(SC, lambda e: e.activation(out=sm[:, 2:3], in_=sm[:, 1:2], func=AF.Exp, scale=-0.5),
             reads=[key("sm", 1)], writes=[key("sm", 2)])
        P.op(V, lambda e: e.scalar_tensor_tensor(out=w1[:], in0=xres[:, tt, :], scalar=sm[:, 2:3], in1=bc1[:],
                                                 op0=ALU.mult, op1=ALU.mult),
             reads=[kxr, key("sm", 2), key("bc1")], writes=[key("w1")])
        P.op(SC, lambda e: e.copy(out=nhi[:], in_=w1[:]), reads=[key("w1")], writes=[key("nhi")])
        P.op(V, lambda e: e.tensor_tensor(out=nlo[:], in0=w1[:], in1=nhi[:], op=ALU.subtract),
             reads=[key("w1"), key("nhi")], writes=[key("nlo")])
        P.dma("sync", d["n2tab"][t0:t0 + 128, :], nhi[:], reads=[key("nhi")], writes=[("n2tab", tt)])
        transpose8(nhi, key("nhi"), nTt[:], key("nTt"))
        transpose8(nlo, key("nlo"), nTlo[:], key("nTlo"))
        P.hold()
        n_ = 0
        for (a_, ka, w_, kw) in ((None, None, wrh, "wrh"), (nTlo, "nTlo", wrh, "wrh"), (None, None, wrl, "wrl")):
            for kc in range(8):
                lhs = nTt[:, kc, :] if a_ is None else a_[:, kc, :]
                P.op(T, lambda e, lhs=lhs, w_=w_, kc=kc, n_=n_: e.matmul(misc[:, 0:36], lhsT=lhs, rhs=w_[:, kc, :],
                                                                         start=(n_ == 0), stop=(n_ == 23)),
                     reads=[key("nTt"), key("nTlo"), key(kw)], writes=[key("misc")])
                n_ += 1
        P.op(V, lambda e: e.tensor_tensor(out=lgs[:], in0=misc[:, 0:36], in1=brt[:], op=ALU.add),
             reads=[key("misc"), key("brt")], writes=[key("lgs")])
        P.release()
        P.op(V, lambda e: e.tensor_reduce(out=sm[:, 10:11], in_=lgs[:, 0:4], axis=AX.X, op=ALU.max),
             reads=[key("lgs")], writes=[key("sm", 10)])
        P.op(V, lambda e: e.tensor_scalar(out=sm[:, 11:12], in0=sm[:, 10:11], scalar1=-1.0, scalar2=None, op0=ALU.mult),
             reads=[key("sm", 10)], writes=[key("sm", 11)])
        P.op(SC, lambda e: e.activation(out=ge[:], in_=lgs[:, 0:4], func=AF.Exp, bias=sm[:, 11:12],
                                        accum_out=sm[:, 12:13]),
             reads=[key("lgs"), key("sm", 11)], writes=[key("ge"), key("sm", 12)])
        P.op(V, lambda e: e.reciprocal(out=sm[:, 13:14], in_=sm[:, 12:13]), reads=[key("sm", 12)],
             writes=[key("sm", 13)])
        P.op(V, lambda e: e.tensor_scalar(out=oh[:], in0=lgs[:, 0:4], scalar1=sm[:, 10:11], scalar2=None,
                                          op0=ALU.is_equal),
             reads=[key("lgs"), key("sm", 10)], writes=[key("oh")])
        P.op(V, lambda e: e.tensor_scalar(out=el8[:], in0=lgs[:, 4:12], scalar1=oh[:, 0:1], scalar2=None, op0=ALU.mult),
             reads=[key("lgs"), key("oh")], writes=[key("el8")])
        for g in range(1, 4):
            P.op(V, lambda e, g=g: e.scalar_tensor_tensor(out=el8[:], in0=lgs[:, 4 + 8 * g:12 + 8 * g],
                                                          scalar=oh[:, g:g + 1], in1=el8[:], op0=ALU.mult, op1=ALU.add),
                 reads=[key("lgs"), key("oh"), key("el8")], writes=[key("el8")])
        P.op(V, lambda e: e.max(out=m8[:], in_=el8[:]), reads=[key("el8")], writes=[key("m8")])
        P.op(V, lambda e: e.tensor_scalar(out=sel8[:], in0=el8[:], scalar1=m8[:, 1:2], scalar2=None, op0=ALU.is_ge),
             reads=[key("el8"), key("m8")], writes=[key("sel8")])
        P.op(V, lambda e: e.tensor_scalar(out=sm[:, 14:15], in0=m8[:, 0:1], scalar1=-1.0, scalar2=None, op0=ALU.mult),
             reads=[key("m8")], writes=[key("sm", 14)])
        P.op(SC, lambda e: e.activation(out=ee8[:], in_=el8[:], func=AF.Exp, bias=sm[:, 14:15]),
             reads=[key("el8"), key("sm", 14)], writes=[key("ee8")])
        P.op(V, lambda e: e.tensor_tensor(out=ee8[:], in0=ee8[:], in1=sel8[:], op=ALU.mult),
             reads=[key("ee8"), key("sel8")], writes=[key("ee8")])
        P.op(V, lambda e: e.tensor_reduce(out=sm[:, 15:16], in_=ee8[:], axis=AX.X, op=ALU.add),
             reads=[key("ee8")], writes=[key("sm", 15)])
        P.op(V, lambda e: e.reciprocal(out=sm[:, 15:16], in_=sm[:, 15:16]), reads=[key("sm", 15)],
             writes=[key("sm", 15)])
        P.op(V, lambda e: e.tensor_tensor(out=sm[:, 15:16], in0=sm[:, 15:16], in1=sm[:, 13:14], op=ALU.mult),
             reads=[key("sm", 15), key("sm", 13)], writes=[key("sm", 15)])
        P.op(V, lambda e: e.tensor_scalar(out=w8[:], in0=ee8[:], scalar1=sm[:, 15:16], scalar2=None, op0=ALU.mult),
             reads=[key("ee8"), key("sm", 15)], writes=[key("w8")])
        for g in range(4):
            P.op(V, lambda e, g=g: e.tensor_scalar(out=wfull[:, 8 * g:8 * g + 8], in0=w8[:], scalar1=oh[:, g:g + 1],
                                                   scalar2=None, op0=ALU.mult),
                 reads=[key("w8"), key("oh")], writes=[key("wfull", g)])
            P.op(V, lambda e, g=g: e.tensor_scalar(out=Aoh[:, 8 * g:8 * g + 8], in0=sel8[:], scalar1=oh[:, g:g + 1],
                                                   scalar2=None, op0=ALU.mult),
                 reads=[key("sel8"), key("oh")], writes=[key("Aoh", g)])
        P.op(G, lambda e: e.tensor_copy(out=Ab[:], in_=Aoh[:]), reads=[key("Aoh")], writes=[key("Ab")])
        P.hold()
        P.op(T, lambda e: e.matmul(misc[:, 64:96], lhsT=triS[:], rhs=Ab[:], start=True, stop=True),
             reads=[key("triS"), key("Ab")], writes=[key("misc")])
        P.op(T, lambda e: e.matmul(misc[:, 96:128], lhsT=ones_b[:], rhs=Ab[:], start=True, stop=True),
             reads=[key("ones_b"), key("Ab")], writes=[key("misc")])
        P.op(V, lambda e: e.tensor_tensor(out=rank[:], in0=misc[:, 64:96], in1=cnt[:], op=ALU.add),
             reads=[key("misc"), key("cnt")], writes=[key("rank")])
        P.op(V, lambda e: e.tensor_tensor(out=cnt[:], in0=misc[:, 96:128], in1=cnt[:], op=ALU.add),
             reads=[key("misc"), key("cnt")], writes=[key("cnt")])
        P.release()
        P.op(V, lambda e: e.tensor_scalar(out=tmp32[:], in0=rank[:], scalar1=float(CAP), scalar2=None, op0=ALU.is_lt),
             reads=[key("rank")], writes=[key("tmp32")])
        P.op(V, lambda e: e.tensor_tensor(out=tmp32[:], in0=tmp32[:], in1=Aoh[:], op=ALU.mult),
             reads=[key("tmp32"), key("Aoh")], writes=[key("tmp32")])
        P.op(V, lambda e: e.tensor_tensor(out=val[:], in0=rank[:], in1=ec[:], op=ALU.add),
             reads=[key("rank"), key("ec")], writes=[key("val")])
        P.op(V, lambda e: e.tensor_tensor(out=val[:], in0=val[:], in1=tmp32[:], op=ALU.mult),
             reads=[key("val"), key("tmp32")], writes=[key("val")])
        P.op(V, lambda e: e.max(out=m8v[:], in_=val[:]), reads=[key("val")], writes=[key("m8v")])
        for k in range(2):
            P.op(V, lambda e, k=k: e.tensor_scalar(out=tmp32[:], in0=val[:], scalar1=m8v[:, k:k + 1], scalar2=None,
                                                   op0=ALU.is_equal),
                 reads=[key("val"), key("m8v")], writes=[key("tmp32")])
            P.op(V, lambda e: e.tensor_tensor(out=tmp32[:], in0=tmp32[:], in1=wfull[:], op=ALU.mult),
                 reads=[key("tmp32"), key("wfull")], writes=[key("tmp32")])
            P.op(V, lambda e, k=k, tt=tt: e.tensor_reduce(out=w12[:, tt, k:k + 1], in_=tmp32[:], axis=AX.X, op=ALU.add),
                 reads=[key("tmp32")], writes=[key("w12", tt, k)])
        P.op(V, lambda e: e.tensor_scalar(out=posf[:], in0=m8v[:, 0:2], scalar1=-1.0, scalar2=None, op0=ALU.add),
             reads=[key("m8v")], writes=[key("posf")])
        P.op(V, lambda e: e.tensor_scalar(out=negf[:], in0=posf[:], scalar1=0.0, scalar2=None, op0=ALU.is_lt),
             reads=[key("posf")], writes=[key("negf")])
        P.op(V, lambda e: e.scalar_tensor_tensor(out=posf[:], in0=negf[:], scalar=dcol[:, 0:1], in1=posf[:],
                                                 op0=ALU.mult, op1=ALU.add),
             reads=[key("negf"), key("dcol"), key("posf")], writes=[key("posf")])
        P.op(V, lambda e, tt=tt: e.tensor_copy(out=posi[:, tt, :], in_=posf[:]), reads=[key("posf")],
             writes=[key("posi", tt)])
        for k in range(2):
            P.op(G, lambda e, k=k, tt=tt: e.indirect_dma_start(
                out=d["tab"][:, :], out_offset=bass.IndirectOffsetOnAxis(ap=posi[:, tt, k:k + 1], axis=0),
                in_=tidt[:, tt, :], in_offset=None),
                reads=[key("posi", tt), key("tidt"), "tab"], writes=[("tab", "sc", tt, k)], dma=True)


    if gather:
        loads(0)
        loads(1)
    for tt in range(0, NTL, 2):
        if gather and tt % 4 == 0 and tt // 4 + 1 < NTL // 4:
            group_gathers(tt // 4 + 1)
        if gather:
            if tt + 2 < NTL:
                loads(tt + 2)
                loads(tt + 3)
        else:
            loads(tt)
            loads(tt + 1)
        P.capture(automark=True)
        t1_tile(tt)
        sa_ = P.end_capture()
        P.capture(automark=True)
        t1_tile(tt + 1)
        sb_ = P.end_capture()
        P.replay([sa_, sb_])

    def load_idx(e_):
        b_ = e_ % 2
        P.dma("sync", idxe[b_][:], d["tab"][e_ * CAP:(e_ + 1) * CAP, :].rearrange("(s p) w -> p s w", p=128),
              reads=["tab"], writes=[key("idxe", b_)])
        for s_ in range(4):
            P.op(G, lambda e, s_=s_, b_=b_: e.indirect_dma_start(
                out=Xg[b_][:, s_, :], out_offset=None, in_=d["n2tab"][:, :],
                in_offset=bass.IndirectOffsetOnAxis(ap=idxe[b_][:, s_, 0:1], axis=0)),
                reads=[key("idxe", b_), "n2tab"], writes=[key("Xg", b_, s_)], dma=True)

    pT2 = misc[:].bitcast(BF16).rearrange("p (k t) -> p k t", k=8)
    pTs_ = [(pT[:], key("pT")), (pT2, key("misc")),
            (Y[0][:].bitcast(BF16).rearrange("p (k t) -> p k t", k=8), key("Y", 0)),
            (Y[1][:].bitcast(BF16).rearrange("p (k t) -> p k t", k=8), key("Y", 1))]

    def emit_T(e_):
        b = e_ % 2
        for s_ in range(4):
            pt_ps, kpt_ps = pTs_[s_ % 4]
            for kc in range(8):
                P.op(T, lambda e, kc=kc, s_=s_, b=b, pt_ps=pt_ps: e.transpose(
                    out=pt_ps[:, kc, :], in_=Xg[b][:, s_, kc * 128:(kc + 1) * 128], identity=ident_b[:]),
                    reads=[key("Xg", b, s_), key("ident_b")], writes=[kpt_ps])
            if s_ % 2 == 0:
                P.op(V, lambda e, s_=s_, pt_ps=pt_ps: e.tensor_copy(out=XT1[:, :, s_ * 128:(s_ + 1) * 128], in_=pt_ps),
                     reads=[kpt_ps], writes=[key("XT", s_)])
            else:
                P.op(SC, lambda e, s_=s_, pt_ps=pt_ps: e.copy(out=XT1[:, :, s_ * 128:(s_ + 1) * 128], in_=pt_ps),
                     reads=[kpt_ps], writes=[key("XT", s_)])

    def emit_h(e_):
        b = e_ % 2
        for fc in range(4):
            for (W_, kw, PS, kp) in ((eg[b], key("eg", b), A[fc % 2], key("A", fc % 2)),
                                     (eu[b], key("eu", b), B[fc % 2], key("B", fc % 2))):
                for kc in range(8):
                    P.op(T, lambda e, W_=W_, PS=PS, kc=kc, fc=fc: e.matmul(
                        PS[:], lhsT=W_[:, kc, fc * 128:(fc + 1) * 128], rhs=XT1[:, kc, :],
                        start=(kc == 0), stop=(kc == 7)),
                        reads=[kw, key("XT")], writes=[kp])
            P.op(SC, lambda e, fc=fc: e.activation(out=sgt[fc % 2][:], in_=A[fc % 2][:], func=AF.Silu),
                 reads=[key("A", fc % 2)], writes=[key("sgt", fc % 2)])
            P.op(V, lambda e, fc=fc: e.tensor_tensor(out=hT[:, fc, :], in0=B[fc % 2][:], in1=sgt[fc % 2][:],
                                                     op=ALU.mult),
                 reads=[key("B", fc % 2), key("sgt", fc % 2)], writes=[key("hT", fc)])

    w1b = w1[:].bitcast(BF16)
    w2b = w2[:].bitcast(BF16)
    ysb_pool = [(ysb[0][:], key("ysb", 0)), (ysb[1][:], key("ysb", 1)),
                (w1b[:, 0:1024], key("w1", "a")), (w1b[:, 1024:2048], key("w1", "b")),
                (w2b[:, 0:1024], key("w2", 0)), (w2b[:, 1024:2048], key("w2", 1))]

    def emit_y(e_):
        b = e_ % 2
        for s_ in range(4):
            yb, kyb = ysb_pool[(e_ * 4 + s_) % len(ysb_pool)]
            YB, ynm = ((Y, "Y"), (A, "A"), (B, "B"))[s_ % 3]
            for half in range(2):
                for fc in range(4):
                    P.op(T, lambda e, fc=fc, s_=s_, half=half, b=b, YB=YB: e.matmul(
                        YB[half][:], lhsT=hT[:, fc, s_ * 128:(s_ + 1) * 128],
                        rhs=ed[b][:, fc, half * 512:(half + 1) * 512], start=(fc == 0), stop=(fc == 3)),
                        reads=[key("hT"), key("ed", b)], writes=[key(ynm, half)])
                if half == 0:
                    P.op(SC, lambda e, yb=yb, YB=YB: e.copy(out=yb[:, 0:512], in_=YB[0][:]), reads=[key(ynm, 0)],
                         writes=[kyb + (0,)])
                else:
                    P.op(V, lambda e, yb=yb, YB=YB: e.tensor_copy(out=yb[:, 512:1024], in_=YB[1][:]),
                         reads=[key(ynm, 1)], writes=[kyb + (1,)])
            r0 = e_ * CAP + s_ * 128
            P.dma("sync", d["ytab"][r0:r0 + 128, :], yb, reads=[kyb], writes=[("ytab", e_, s_)])

    load_idx(0)
    emit_T(0)
    for e_ in range(nexp):
        if e_ + 1 < nexp:
            load_idx(e_ + 1)
            load_expert(e_ + 1)
        emit_h(e_)
        if e_ + 1 < nexp:
            emit_T(e_ + 1)
        emit_y(e_)
    P.dma("sync", bc1[:], d["ple_norm"].partition_broadcast(128), writes=[key("bc1")])
    P.dma("sync", bc2[:], d["b_ple"].partition_broadcast(128), writes=[key("bc2")])
    P.dma("gpsimd", wbig[:], d["wpg"].rearrange("(c p) n -> p c n", p=128), writes=[key("wbig")])
    pTs2 = sb([128, 2, 128], BF16, "pTs2")
    w3 = XT1[:].rearrange("p k t -> p (k t)").bitcast(F32)[:, 0:1024]
    nT3 = [nTlo, nTt]
    knT3 = [key("nTlo"), key("nTt")]
    pTsl = [pTs, pTs2]
    kpTs = [key("pTs"), key("pTs2")]

    def gback(tt):
        for k in range(2):
            gb = Xg[tt % 2]
            kgb = key("Xg", tt % 2, k)
            P.op(G, lambda e, k=k, gb=gb: e.indirect_dma_start(
                out=gb[:, k, :], out_offset=None, in_=d["ytab"][:, :],
                in_offset=bass.IndirectOffsetOnAxis(ap=posi[:, tt, k:k + 1], axis=0)),
                reads=[key("posi", tt), "ytab"], writes=[kgb], dma=True)
            P.op(V, lambda e, k=k, gb=gb: e.scalar_tensor_tensor(
                out=xres[:, tt, :], in0=gb[:, k, :], scalar=w12[:, tt, k:k + 1], in1=xres[:, tt, :], op0=ALU.mult,
                op1=ALU.add),
                reads=[kgb, key("w12", tt), key("xres", tt)], writes=[key("xres", tt)])

    hTf = hT[:].rearrange("p a b -> p (a b)").bitcast(F32)
    t3 = [dict(w1=w1[:], kw1=key("w1"), junk=nlo[:], kjunk=key("nlo"), nhi=nhi[:], knhi=key("nhi"), sm0=0,
               pt=pt_[:], kpt=key("pt"), pb=pb[:], kpb=key("pb"), ptr=pT[:], kptr=key("pT"),
               G=A, kG="A", Bk=B[0], kB=key("B", 0)),
          dict(w1=hTf, kw1=key("hT"), junk=ysb[1][:], kjunk=key("ysb", 1), nhi=ysb[0][:], knhi=key("ysb", 0), sm0=4,
               pt=sgt[0][:, 0:256], kpt=key("sgt", 0), pb=sgt[1][:].bitcast(BF16)[:, 0:256], kpb=key("sgt", 1),
               ptr=misc[:].bitcast(BF16).rearrange("p (k t) -> p k t", k=8), kptr=key("misc"),
               G=Y, kG="Y", Bk=B[1], kB=key("B", 1))]

    def tile_ops(tt):
        t0 = tt * 128
        par = tt % 2
        q = t3[par]
        c0_ = q["sm0"]
        x_ap = xres[:, tt, :]
        kx = key("xres", tt)
        wo = w2 if par == 0 else w3
        kwo = 'w2' if par == 0 else 'XT'
        P.op(SC, lambda e: e.activation(out=q["junk"], in_=x_ap, func=AF.Square, accum_out=sm[:, c0_:c0_ + 1]),
             reads=[kx], writes=[q["kjunk"], key("sm", c0_)])
        P.mark()
        P.op(SC, lambda e: e.activation(out=sm[:, c0_ + 1:c0_ + 2], in_=sm[:, c0_:c0_ + 1], func=AF.Ln,
                                        scale=1.0 / 1024, bias=eps[:]),
             reads=[key("sm", c0_), key("eps")], writes=[key("sm", c0_ + 1)])
        P.op(SC, lambda e: e.activation(out=sm[:, c0_ + 2:c0_ + 3], in_=sm[:, c0_ + 1:c0_ + 2], func=AF.Exp, scale=-0.5),
             reads=[key("sm", c0_ + 1)], writes=[key("sm", c0_ + 2)])
        P.dma("sync", q["pt"], d["p"][t0:t0 + 128, :], writes=[q["kpt"]])
        P.mark()
        P.op(V, lambda e: e.scalar_tensor_tensor(out=q["w1"], in0=x_ap, scalar=sm[:, c0_ + 2:c0_ + 3], in1=bc1[:],
                                                 op0=ALU.mult, op1=ALU.mult),
             reads=[kx, key("sm", c0_ + 2), key("bc1")], writes=[q["kw1"]])
        P.op(G, lambda e: e.tensor_copy(out=q["pb"], in_=q["pt"]), reads=[q["kpt"]], writes=[q["kpb"]])
        P.mark()
        P.op(SC, lambda e: e.copy(out=q["nhi"], in_=q["w1"]), reads=[q["kw1"]], writes=[q["knhi"]])
        P.mark()
        for kc in range(8):
            P.op(T, lambda e, kc=kc: e.transpose(out=q["ptr"][:, kc, :], in_=q["nhi"][:, kc * 128:(kc + 1) * 128],
                                                 identity=ident_b[:]),
                 reads=[q["knhi"], key("ident_b")], writes=[q["kptr"]])
        P.mark()
        P.op(V, lambda e: e.tensor_copy(out=nT3[par][:], in_=q["ptr"]), reads=[q["kptr"]], writes=[knT3[par]])
        P.mark()
        for kc in range(2):
            P.op(T, lambda e, kc=kc: e.transpose(out=q["ptr"][:, kc, :], in_=q["pb"][:, kc * 128:(kc + 1) * 128],
                                                 identity=ident_b[:]),
                 reads=[q["kpb"], key("ident_b")], writes=[q["kptr"]])
        P.mark()
        P.op(SC, lambda e: e.copy(out=pTsl[par][:], in_=q["ptr"][:, 0:2, :]), reads=[q["kptr"]], writes=[kpTs[par]])
        P.mark()
        for half in range(2):
            for kc in range(8):
                P.op(T, lambda e, kc=kc, half=half: e.matmul(q["G"][half][:], lhsT=nT3[par][:, kc, :],
                                                             rhs=wbig[:, kc, half * 512:(half + 1) * 512],
                                                             start=(kc == 0), stop=(kc == 7)),
                     reads=[knT3[par], key("wbig")], writes=[key(q["kG"], half)])
            P.mark()
            P.op(V, lambda e, half=half: e.tensor_tensor(out=wo[:, half * 512:(half + 1) * 512], in0=q["G"][half][:],
                                                         in1=bc2[:, half * 512:(half + 1) * 512], op=ALU.add),
                 reads=[key(q["kG"], half), key("bc2")], writes=[key(kwo, half)])
            P.mark()
        P.op(SC, lambda e: e.activation(out=wo[:], in_=wo[:], func=AF.Sigmoid), reads=[key(kwo)], writes=[key(kwo)])
        P.mark()
        for half in range(2):
            for kc in range(2):
                P.op(T, lambda e, kc=kc, half=half: e.matmul(q["Bk"][:], lhsT=pTsl[par][:, kc, :],
                                                             rhs=wpp[:, kc, half * 512:(half + 1) * 512],
                                                             start=(kc == 0), stop=(kc == 1)),
                     reads=[kpTs[par], key("wpp")], writes=[q["kB"]])
            P.mark()
            P.op(V, lambda e, half=half: e.tensor_tensor(out=wo[:, half * 512:(half + 1) * 512], in0=q["Bk"][:],
                                                         in1=wo[:, half * 512:(half + 1) * 512], op=ALU.mult),
                 reads=[q["kB"], key(kwo)], writes=[key(kwo, half)])
            P.mark()
        P.op(G, lambda e: e.tensor_tensor(out=wo[:], in0=wo[:], in1=xres[:, tt, :], op=ALU.add),
             reads=[key(kwo), key("xres", tt)], writes=[key(kwo)])
        P.mark()
        P.dma("sync", d["xo"][t0:t0 + 128, :], wo[:], reads=[key(kwo)], writes=[key("xo")])
        if after_tile is not None:
            after_tile(tt, key("xo"))
        P.mark()

    gback(0)
    gback(1)
    for tt in range(0, NTL, 2):
        if tt + 2 < NTL:
            gback(tt + 2)
            gback(tt + 3)
        P.capture()
        tile_ops(tt)
        s0_ = P.end_capture()
        P.capture()
        tile_ops(tt + 1)
        s1_ = P.end_capture()
        P.replay([s0_, s1_])
    return [key("xo")]


def mixer_inputs(I, L, b, g, xb):
    w_in = I["w_in"][L]
    r0 = g * 256
    cols = np.concatenate([np.arange(r0, r0 + 256), 512 + np.arange(r0, r0 + 256), 1024 + np.arange(r0, r0 + 256),
                           1536 + np.arange(r0, r0 + 256), 2048 + np.arange(r0, r0 + 256), 2560 + 4 * g + np.arange(4)])
    win = np.ascontiguousarray(w_in[:, cols])
    pv = np.zeros((128, NPV), np.float32)
    for c in range(2):
        ch = slice(r0 + c * 128, r0 + (c + 1) * 128)
        for j in range(4):
            pv[:, j * 2 + c] = I["conv_w"][L][j, ch]
        pv[:, 8 + c] = I["conv_b"][L][ch]
        pv[:, 10 + c] = I["b_rgate"][L][ch]
        pv[:, 12 + c] = I["b_igate"][L][ch]
        pv[:, 14 + c] = I["lru_lambda"][L][ch]
        pv[:, 16 + c] = I["lru_out_norm"][L][ch]
    pv[:64, 18] = I["q_norm"][L]
    pv[64:, 18] = I["q_norm"][L]
    pv[:64, 19] = I["k_norm"][L]
    pv[64:, 19] = I["k_norm"][L]
    for hl in range(4):
        pv[:64, 20 + hl] = I["att_out_norm"][L][(4 * g + hl) * 64:(4 * g + hl + 1) * 64]
    wr_bd = np.zeros((2, 128, 128), np.float32)
    wi_bd = np.zeros((2, 128, 128), np.float32)
    for c in range(2):
        for k in range(2):
            blk = 4 * g + 2 * c + k
            wr_bd[c, k * 64:(k + 1) * 64, k * 64:(k + 1) * 64] = I["w_rgate"][L][blk]
            wi_bd[c, k * 64:(k + 1) * 64, k * 64:(k + 1) * 64] = I["w_igate"][L][blk]
    return dict(xb=np.ascontiguousarray(xb), win=win, mixn=np.ascontiguousarray(I["mix_norm"][L]), pv=pv,
                wr_bd=wr_bd, wi_bd=wi_bd, bf=np.ascontiguousarray(I["b_forget"][L][4 * g:4 * g + 4]))

def token_inputs(I, L, x_tok, yT2, ss2, p_tok):
    return dict(x=np.ascontiguousarray(x_tok),
                p=np.ascontiguousarray(p_tok), wout=I["w_out"][L], ffn_norm=I["ffn_norm"][L], ple_norm=I["ple_norm"][L],
                b_ple=I["b_ple_gate"][L], wrt=np.ascontiguousarray(np.concatenate([I["w_group"][L], I["w_router"][L]], 1)),
                brt=np.concatenate([I["b_group"][L], I["b_router"][L]]), weg=I["w_exp_gate"][L], weu=I["w_exp_up"][L],
                wed=I["w_exp_down"][L], wpg=I["w_ple_gate"][L], wpp=I["w_ple_proj"][L])


NT_CORE = 2048
RG = [[0, 1], [2, 3], [4, 5], [6, 7]]
MIX_IN = [("win", [1024, 1284]), ("mixn", [1024]), ("pv", [128, NPV]), ("wr_bd", [2, 128, 128]), ("wi_bd", [2, 128, 128]), ("bf", [4])]
TOK_IN = [("p", [NT_CORE, 256]), ("wout", [1024, 1024]), ("ffn_norm", [1024]), ("ple_norm", [1024]), ("b_ple", [1024]),
          ("wrt", [1024, 36]), ("brt", [36]), ("weg", [32, 1024, 512]), ("weu", [32, 1024, 512]), ("wed", [32, 512, 1024]),
          ("wpg", [1024, 1024]), ("wpp", [256, 1024])]


def build_fused(nlayers=2):
    nc = bass.Bass("TRN2", target_bir_lowering=False)
    ext = lambda name, shape, dt=F32: nc.dram_tensor(name, shape, dt, kind="ExternalInput").ap()
    xb = ext("xb", [4096, 1024])
    xh = ext("xh", [NT_CORE, 1024])
    idx_y4 = ext("idx_y4", [128, 32], I32)
    idx_s = ext("idx_s", [128, 2 * 16], I32)
    out = nc.dram_tensor("out", [NT_CORE, 1024], F32, kind="ExternalOutput").ap()
    sems = Sems(nc)
    x_tile, x_half = None, xh
    for L in range(nlayers):
        dm = {n: ext(f"{n}_{L}", sh) for n, sh in MIX_IN}
        dm["xb"] = xb
        if x_tile is not None:
            dm["xb_tile"] = x_tile
        yT32 = nc.dram_tensor(f"yT_{L}", [512, 2048], F32).ap()
        ss = nc.dram_tensor(f"ss_{L}", [4096, 2], F32).ap()
        yTg32 = nc.dram_tensor(f"yTg_{L}", [1024, 2048], F32).ap()
        ssg = nc.dram_tensor(f"ssg_{L}", [2 * 4096, 2], F32).ap()
        dm["yT"], dm["ss"] = yT32.bitcast(BF16), ss
        P = Prog(nc, sems)
        outs = mixer_phase(P, nc, dm, pfx=f"m{L}")
        for k in range(2):
            P.op("gpsimd", lambda e, k=k: e.collective_compute(
                "AllGather", ALU.bypass, replica_groups=RG, ins=[yT32[k * 256:(k + 1) * 256, :]],
                outs=[yTg32[k * 512:(k + 1) * 512, :]]), reads=outs, writes=[(f"yTg{L}", k)], cc=True)
        P.op("gpsimd", lambda e: e.collective_compute("AllGather", ALU.bypass, replica_groups=RG, ins=[ss[:, :]],
                                                      outs=[ssg[:, :]]),
             reads=outs, writes=[f"ssg{L}"], cc=True)
        P.emit()
        dt_ = {n: ext(f"{n}_{L}", sh) for n, sh in TOK_IN}
        dt_["n2tab"] = nc.dram_tensor(f"n2tab_{L}", [NT_CORE, 1024], BF16).ap()
        dt_["tab"] = nc.dram_tensor(f"tab_{L}", [NSLOT + 128, TABW], I32).ap()
        dt_["ytab"] = nc.dram_tensor(f"ytab_{L}", [NSLOT + 128, 1024], BF16).ap()
        dt_.update(x=x_half, yTg4=yTg32.bitcast(BF16).rearrange("r (b t) -> (r b) t", t=512), ssg=ssg, idx_y4=idx_y4,
                   idx_s=idx_s)
        last = (L == nlayers - 1)
        xo = out if last else nc.dram_tensor(f"xo_{L}", [NT_CORE, 1024], F32).ap()
        dt_["xo"] = xo
        P = Prog(nc, sems)
        if last:
            outs = token_phase(P, nc, dt_, NT=NT_CORE, pfx=f"t{L}", gather=True)
            P.finish_wait("sync", outs)
        else:
            xg = nc.dram_tensor(f"xg_{L}", [4096, 1024], F32).ap()

            def after_tile(tt, kxo, P=P, xo=xo, xg=xg, L=L):
                if tt % 4 == 3:
                    k = tt // 4
                    P.op("gpsimd", lambda e: e.collective_compute(
                        "AllGather", ALU.bypass, replica_groups=RG, ins=[xo[k * 512:(k + 1) * 512, :]],
                        outs=[xg[k * 1024:(k + 1) * 1024, :]]), reads=[kxo], writes=[(f"xg{L}", k)], cc=True)
            outs = token_phase(P, nc, dt_, NT=NT_CORE, pfx=f"t{L}", gather=True, after_tile=after_tile)

            def x_tile(tt, xg=xg):
                s0 = tt * 128
                h, k, i = s0 // 2048, (s0 % 2048) // 512, s0 % 512
                r0 = k * 1024 + h * 512 + i
                return xg[r0:r0 + 128, :]
            x_half = xo
        P.emit()
    sems.close()
    return nc

def _core_inputs(I, c, nlayers=2):
    b, r = c // 2, c % 2
    sl = slice(r * NT_CORE, (r + 1) * NT_CORE)
    m = dict(xb=np.ascontiguousarray(I["x"][b]), xh=np.ascontiguousarray(I["x"][b, sl]))
    p_ = np.arange(128, dtype=np.int64)
    iy = np.zeros((128, 32), np.int64)
    isx = np.zeros((128, 2 * 16), np.int64)
    for g in range(2):
        for cc in range(4):
            k = cc // 2
            for grp in range(4):
                iy[:, (g * 4 + cc) * 4 + grp] = (k * 512 + g * 256 + (cc % 2) * 128 + p_) * 8 + r * 4 + grp
        for tt in range(16):
            isx[:, g * 16 + tt] = g * 4096 + r * 2048 + tt * 128 + p_
    m["idx_y4"] = iy.astype(np.int32)
    m["idx_s"] = isx.astype(np.int32)
    for L in range(nlayers):
        mi = mixer_inputs(I, L, b, r, I["x"][b])
        for n, _ in MIX_IN:
            m[f"{n}_{L}"] = np.ascontiguousarray(mi[n])
        ti = token_inputs(I, L, I["x"][b, sl], None, None, I["p"][L][b][sl])
        for n, _ in TOK_IN:
            m[f"{n}_{L}"] = np.ascontiguousarray(ti[n])
    return m


def kernel(**inputs):
    I = {k: np.asarray(v) for k, v in inputs.items()}
    cores = list(range(8))
    nc = build_fused(2)
    maps = [_core_inputs(I, c) for c in cores]
    res = run_bass_kernel_spmd(nc, maps, core_ids=cores)
    out = np.empty(I["x"].shape, np.float32)
    for c in cores:
        b, r = c // 2, c % 2
        out[b, r * NT_CORE:(r + 1) * NT_CORE] = np.asarray(res.results[c]["out"])
    return out
```

```python
import numpy as np
from contextlib import ExitStack
import concourse.bass as bass
import concourse.mybir as mybir
from concourse.bass_utils import run_bass_kernel_spmd

F32 = mybir.dt.float32
BF16 = mybir.dt.bfloat16
I32 = mybir.dt.int32
U32 = mybir.dt.uint32
AF = mybir.ActivationFunctionType
ALU = mybir.AluOpType
AX = mybir.AxisListType

ENGS = ("sync", "scalar", "vector", "gpsimd", "tensor")
NPOOL = 56
NPOOL_HW = 28
SAME_ENGINE_SYNC = True


def _conf(a, b):
    n = min(len(a), len(b))
    return a[:n] == b[:n]


class _Rec:
    def __init__(self):
        self.call = None

    def __getattr__(self, name):
        def f(*a, **k):
            assert self.call is None
            self.call = (name, a, k)
            return self
        return f


class Sems:
    def __init__(self, nc):
        self.stack = ExitStack()
        st = self.stack
        self.esem = {e: st.enter_context(nc.semaphore(f"es_{e}")) for e in ENGS}
        self.pool = [st.enter_context(nc.semaphore(f"dp_{i}")) for i in range(NPOOL)]
        self.cc = st.enter_context(nc.semaphore("ccs"))
        self.bar = st.enter_context(nc.semaphore("bars"))
        self.ecount = {e: 0 for e in ENGS}
        self.pcount = [0] * NPOOL
        self.cccount = 0
        self.nphase = 0
        self.ndma = 0
        self.ndma_sw = 0

    def close(self):
        self.stack.close()


class Prog:
    def __init__(self, nc, sems=None):
        self.nc = nc
        self.sems = sems
        self.ops = []
        self.track = {}
        self.stack = ExitStack()
        self.ntens = 0

    def sb(self, shape, dt, name=None):
        self.ntens += 1
        name = name or f"t{self.ntens}"
        return self.stack.enter_context(self.nc.sbuf_tensor(name, list(shape), dt))

    def ps(self, shape, dt, name=None):
        self.ntens += 1
        name = name or f"p{self.ntens}"
        return self.stack.enter_context(self.nc.psum_tensor(name, list(shape), dt))

    def _deps(self, reads, writes):
        deps = set()
        for k in reads:
            k = k if isinstance(k, tuple) else (k,)
            for sk, ent in self.track.get(k[0], {}).items():
                if _conf(sk, k) and ent[0] is not None:
                    deps.add(ent[0])
        for k in writes:
            k = k if isinstance(k, tuple) else (k,)
            for sk, ent in self.track.get(k[0], {}).items():
                if _conf(sk, k):
                    if ent[0] is not None:
                        deps.add(ent[0])
                    deps.update(ent[1])
        return deps

    def _commit(self, idx, reads, writes):
        for k in reads:
            k = k if isinstance(k, tuple) else (k,)
            d = self.track.setdefault(k[0], {})
            if k not in d:
                lw = None
                for sk, ent in d.items():
                    if _conf(sk, k) and ent[0] is not None:
                        lw = ent[0] if lw is None else max(lw, ent[0])
                d[k] = [lw, []]
            d[k][1].append(idx)
        for k in writes:
            k = k if isinstance(k, tuple) else (k,)
            d = self.track.setdefault(k[0], {})
            for sk in [sk for sk in d if _conf(sk, k) and sk != k]:
                if len(sk) > len(k):
                    del d[sk]
                else:
                    d[sk] = [idx, []]
            d[k] = [idx, []]

    def capture(self, automark=False):
        self._cap = []
        self._automark = automark

    def hold(self):
        self._hold = getattr(self, "_hold", 0) + 1

    def release(self):
        self._hold -= 1
        if self._hold == 0:
            self.mark()

    def mark(self):
        if getattr(self, "_cap", None) is not None:
            self._cap.append(None)

    def end_capture(self):
        c, self._cap = self._cap, None
        return c

    def replay(self, streams):
        units = []
        for st in streams:
            us, cur = [], []
            for it in st:
                if it is None:
                    if cur:
                        us.append(cur)
                    cur = []
                else:
                    cur.append(it)
            if cur:
                us.append(cur)
            units.append(us)
        n = max(len(u) for u in units)
        for k in range(n):
            for us in units:
                if k < len(us):
                    for (eng, fn, reads, writes, dma, cc) in us[k]:
                        self._op_now(eng, fn, reads, writes, dma, cc)

    def op(self, eng, fn, reads=(), writes=(), dma=False, cc=False):
        rec = _Rec()
        fn(rec)
        assert rec.call is not None
        call = rec.call
        fn = lambda e, call=call: getattr(e, call[0])(*call[1], **call[2])
        if getattr(self, "_cap", None) is not None:
            self._cap.append((eng, fn, list(reads), list(writes), dma, cc))
            if getattr(self, "_automark", False) and not getattr(self, "_hold", 0):
                self._cap.append(None)
            return None
        return self._op_now(eng, fn, reads, writes, dma, cc)

    def _op_now(self, eng, fn, reads=(), writes=(), dma=False, cc=False):
        idx = len(self.ops)
        deps = self._deps(reads, writes)
        self._commit(idx, reads, writes)
        if getattr(self, "serial", False) and idx > 0:
            deps.add(idx - 1)
        if cc:
            prev = getattr(self, "_last_cc", None)
            if prev is not None:
                deps.add(prev)
            self._last_cc = idx
        self.ops.append(dict(eng=eng, fn=fn, deps=sorted(deps), dma=dma, idx=idx, cc=cc))
        return idx

    def dma(self, eng, out, in_, reads=(), writes=(), **kw):
        return self.op(eng, lambda e: e.dma_start(out=out, in_=in_, **kw), reads, writes, dma=True)

    def emit(self):
        nc = self.nc
        ops = self.ops
        has_dep = [False] * len(ops)
        for o in ops:
            for d in o["deps"]:
                if SAME_ENGINE_SYNC or ops[d]["eng"] != o["eng"] or ops[d]["dma"]:
                    has_dep[d] = True
        own = self.sems is None
        sems = self.sems if self.sems is not None else Sems(nc)
        esem, pool, ecount, pcount = sems.esem, sems.pool, sems.ecount, sems.pcount
        pool_prev = [None] * NPOOL
        sems.nphase += 1
        for o in ops:
            if o["cc"]:
                sems.cccount += 1
                o["sig"] = (sems.cc, sems.cccount, ("cc",))
                o["pool_prev"] = None
            elif o["dma"]:
                if o["eng"] == "gpsimd":
                    s = NPOOL_HW + sems.ndma_sw % (NPOOL - NPOOL_HW)
                    sems.ndma_sw += 1
                else:
                    s = sems.ndma % NPOOL_HW
                    sems.ndma += 1
                pcount[s] += 16
                o["sig"] = (pool[s], pcount[s], ("p", s))
                o["pool_prev"] = pool_prev[s]
                pool_prev[s] = o["idx"]
            elif has_dep[o["idx"]] :
                ecount[o["eng"]] += 1
                o["sig"] = (esem[o["eng"]], ecount[o["eng"]], ("e", o["eng"]))
            else:
                o["sig"] = None
        per = {e: [o for o in ops if o["eng"] == e] for e in ENGS}

        def run(engname, eng):
            waited = {}
            for o in per[engname]:
                deps = list(o["deps"])
                if o["dma"] and o["pool_prev"] is not None:
                    deps.append(o["pool_prev"])
                need = {}
                for d in deps:
                    od = ops[d]
                    if not od["dma"] and not od["cc"] and od["eng"] == engname and not SAME_ENGINE_SYNC:
                        continue
                    if not od["dma"] and not od["cc"] and od["eng"] == "tensor" and engname == "tensor":
                        continue
                    sem, val, key = od["sig"]
                    if waited.get(key, 0) >= val:
                        continue
                    if key not in need or need[key][1] < val:
                        need[key] = (sem, val)
                for key, (sem, val) in need.items():
                    eng.wait_ge(sem, val)
                    waited[key] = val
                ins = o["fn"](eng)
                if o["sig"] is not None:
                    sem, val, key = o["sig"]
                    ins.then_inc(sem, 16 if (o["dma"] and not o["cc"]) else 1)
            for s_ in range(NPOOL):
                if pcount[s_] > 0:
                    eng.wait_ge(pool[s_], pcount[s_])
            if sems.cccount > 0:
                eng.wait_ge(sems.cc, sems.cccount)
            eng.drain().then_inc(sems.bar, 1)
            eng.wait_ge(sems.bar, 5 * sems.nphase)

        with nc.Block() as block:
            @block.sync
            def _(e):
                run("sync", e)

            @block.scalar
            def _(e):
                run("scalar", e)

            @block.vector
            def _(e):
                run("vector", e)

            @block.gpsimd
            def _(e):
                run("gpsimd", e)

            @block.tensor
            def _(e):
                run("tensor", e)
        self.stack.close()
        if own:
            sems.close()

    def finish_wait(self, eng, keys):
        return self.op(eng, lambda e: e.nop(), reads=keys, writes=())

NPV = 24
S = 4096
NCHUNK = 8
EPS = 1e-6


def mixer_phase(P, nc, d, pfx="m", stop=99, nchunk=NCHUNK):
    K = lambda *a: (pfx,) + a

    def key(*a):
        return (pfx + "_" + str(a[0]),) + tuple(a[1:])

    sb = lambda shape, dt, name: P.sb(shape, dt, pfx + "_" + name)
    gmix = sb([128, 1024], F32, "gmix")
    wb = sb([128, 8, 1284], BF16, "wb")
    pv = sb([128, NPV], F32, "pv")
    wr = sb([128, 2, 128], BF16, "wr")
    wi = sb([128, 2, 128], BF16, "wi")
    bfb = sb([128, 4], F32, "bfb")
    ident_b = sb([128, 128], BF16, "ident_b")
    tri_b = sb([128, 128], BF16, "tri_b")
    tri_f = sb([128, 128], F32, "tri_f")
    ones_f = sb([128, 128], F32, "ones_f")
    ones_b = sb([128, 128], BF16, "ones_b")
    ones_bd = sb([128, 128], BF16, "ones_bd")
    eps = sb([128, 1], F32, "eps")
    one1 = sb([128, 1], F32, "one1")
    cl = sb([128, 2, 2], F32, "cl")
    gq8 = sb([128, 1], F32, "gq8")
    xt = [sb([128, 1024], F32, f"xt{i}") for i in range(2)]
    st = [sb([128, 4], F32, f"st{i}") for i in range(2)]
    nb1 = sb([128, 1024], BF16, "nb0")
    nb = [nb1, nb1]
    nT = [sb([128, 8, 512], BF16, f"nT{i}") for i in range(2)]
    zxb = [sb([128, 515], F32, f"zxb{c}") for c in range(2)]
    xc = [sb([128, 512], F32, f"xc{i}") for i in range(2)]
    xcb = [sb([128, 512], BF16, f"xcb{i}") for i in range(2)]
    rr = [sb([128, 512], F32, f"rr{i}") for i in range(2)]
    ig = [sb([128, 512], F32, f"ig{i}") for i in range(2)]
    aa = [sb([128, 512], F32, f"aa{i}") for i in range(2)]
    a2 = [sb([128, 512], F32, f"a2{i}") for i in range(2)]
    sq_ = [sb([128, 512], F32, f"sq_{i}") for i in range(2)]
    uu = [sb([128, 512], F32, f"uu{i}") for i in range(2)]
    hh = [sb([128, 512], F32, f"hh{i}") for i in range(2)]
    hst = sb([128, 2], F32, "hst")
    gg = [sb([128, 512], F32, f"gg{i}") for i in range(2)]
    yy = [sb([128, 512], F32, f"yy{i}") for i in range(2)]
    ysq = [sb([128, 512], BF16, f"ysq{c}") for c in range(2)]
    ygb = [sb([128, 512], BF16, f"ygb{c}") for c in range(2)]
    qsq = [sb([128, 512], BF16, f"qsq{i}") for i in range(2)]
    qlr = [sb([128, 512], F32, f"qlr{i}") for i in range(2)]
    qrs = [sb([128, 512], F32, f"qrs{i}") for i in range(2)]
    q_aug = [sb([65, 4, 512], BF16, f"qaug{i}") for i in range(2)]
    k_aug = sb([65, 4, S], BF16, "kaug")
    Vp = sb([128, 32, 4, 128], BF16, "Vp")
    den_sb = sb([128, 512], F32, "den_sb")
    dhi = sb([128, 512], BF16, "dhi")
    dlo = sb([128, 512], BF16, "dlo")
    shb = sb([128, 64], BF16, "shb")
    nlb = sb([128, 4, 4, 65], BF16, "nlb")
    low_b = sb([128, 128], BF16, "low_b")
    ft = sb([128, 4], F32, "ft")
    fe = sb([128, 4], F32, "fe")
    nl128 = sb([128, 128], F32, "nl128")
    nl = nl128[:, 0:4]
    carry = sb([128, 4], F32, "carry")
    nc_tok = sb([128, 32, 4], F32, "nc_tok")
    cref = [sb([128, 4], F32, f"cref{i}") for i in range(2)]
    bias_t = [sb([128, 32, 4], F32, f"bias{i}") for i in range(2)]
    Pt = [sb([128, 512], BF16, f"Pt{i}") for i in range(4)]
    yat = sb([64, 512], F32, "yat")
    asq = [sb([64, 512], BF16, f"asq{h}") for h in range(4)]
    agb = [sb([64, 512], BF16, f"agb{h}") for h in range(2)]
    sso = sb([128, 4, 2], F32, "sso")
    pa = [P.ps([128, 512], F32, pfx + f"_pa{i}") for i in range(2)]
    dps = P.ps([128, 512], F32, pfx + "_dps")
    pT = P.ps([128, 8, 128], BF16, pfx + "_pT")
    sps = [P.ps([128, 512], F32, pfx + f"_sps{i}") for i in range(2)]
    ops_ = P.ps([128, 512], F32, pfx + "_ops")
    misc = P.ps([128, 512], F32, pfx + "_misc")
    pa_i = [0]

    pa_pool = [(pa[0], key("pa", 0)), (pa[1], key("pa", 1)), (dps, key("dps")), (sps[0], key("sps", 0)),
               (sps[1], key("sps", 1)), (ops_, key("ops"))]

    s_pool = [(sps[0], key("sps", 0)), (sps[1], key("sps", 1)), (pa[0], key("pa", 0)), (pa[1], key("pa", 1))]

    def next_pa():
        i = pa_i[0] % len(pa_pool)
        pa_i[0] += 1
        return pa_pool[i]

    V, SC, G, T = "vector", "scalar", "gpsimd", "tensor"

    P.dma("sync", gmix[:], d["mixn"].partition_broadcast(128), writes=[key("gmix")])
    P.dma("sync", pv[:], d["pv"][:, :], writes=[key("pv")])
    P.dma("sync", bfb[:], d["bf"].partition_broadcast(128), writes=[key("bfb")])
    P.dma("gpsimd", wb[:], d["win"].rearrange("(c p) n -> p c n", p=128), writes=[key("wb")])
    P.dma("gpsimd", wr[:], d["wr_bd"].rearrange("c p n -> p c n"), writes=[key("wr")])
    P.dma("gpsimd", wi[:], d["wi_bd"].rearrange("c p n -> p c n"), writes=[key("wi")])
    P.op(V, lambda e: e.memset(eps[:], EPS), writes=[key("eps")])
    P.op(V, lambda e: e.memset(one1[:], 1.0), writes=[key("one1")])
    P.op(V, lambda e: e.memset(ones_f[:], 1.0), writes=[key("ones_f")])
    P.op(V, lambda e: e.memset(ones_b[:], 1.0), writes=[key("ones_b")])
    P.op(V, lambda e: e.memset(ones_bd[:], 0.0), writes=[key("ones_bd")])
    P.op(V, lambda e: e.memset(ones_bd[0:64, 0:64], 1.0), writes=[key("ones_bd")])
    P.op(V, lambda e: e.memset(ones_bd[64:128, 64:128], 1.0), writes=[key("ones_bd")])
    P.op(G, lambda e: e.affine_select(out=ident_b[:], in_=ones_b[:], pattern=[[1, 128]], compare_op=ALU.is_equal,
                                      fill=0.0, base=0, channel_multiplier=-1),
         reads=[key("ones_b")], writes=[key("ident_b")])
    P.op(G, lambda e: e.affine_select(out=tri_b[:], in_=ones_b[:], pattern=[[1, 128]], compare_op=ALU.is_ge,
                                      fill=0.0, base=0, channel_multiplier=-1),
         reads=[key("ones_b")], writes=[key("tri_b")])
    P.op(G, lambda e: e.affine_select(out=tri_f[:], in_=ones_f[:], pattern=[[1, 128]], compare_op=ALU.is_ge,
                                      fill=0.0, base=0, channel_multiplier=-1),
         reads=[key("ones_f")], writes=[key("tri_f")])
    P.op(V, lambda e: e.memset(k_aug[64:65, :, :], 1.0), writes=[key("kaug", "aug")])
    P.op(G, lambda e: e.affine_select(out=low_b[:], in_=ones_b[:], pattern=[[-1, 128]], compare_op=ALU.is_gt,
                                      fill=0.0, base=0, channel_multiplier=1),
         reads=[key("ones_b")], writes=[key("low_b")])
    P.op(V, lambda e: e.memset(carry[:], 0.0), writes=[key("carry")])
    P.op(G, lambda e: e.memset(Vp[:, :, :, 64:128], 1.0), writes=[key("Vp", "ones")])
    P.op(V, lambda e: e.memset(dhi[:], 0.0), writes=[key("dhi")])
    P.op(V, lambda e: e.memset(dlo[:], 0.0), writes=[key("dlo")])
    P.op(G, lambda e: e.affine_select(out=shb[:], in_=ones_b[:, 0:64], pattern=[[-1, 64]], compare_op=ALU.is_equal,
                                      fill=0.0, base=-64, channel_multiplier=1),
         reads=[key("ones_b")], writes=[key("shb")])
    P.op(V, lambda e: e.memset(nlb[:], 0.0), writes=[key("nlb")])
    P.op(V, lambda e: e.memset(nl128[:], 0.0), writes=[key("nl")])
    P.op(V, lambda e: e.memset(hst[:], 0.0), writes=[key("hst")])
    for c in range(2):
        P.op(V, lambda e, c=c: e.memset(zxb[c][:, 0:3], 0.0), writes=[key("zxb", c, "halo")])
    P.op(SC, lambda e: e.activation(out=cl[:, :, 0], in_=pv[:, 14:16], func=AF.Exp, scale=-1.0),
         reads=[key("pv")], writes=[key("cl")])
    P.op(SC, lambda e: e.activation(out=cl[:, :, 1], in_=cl[:, :, 0], func=AF.Ln, bias=one1[:]),
         reads=[key("cl"), key("one1")], writes=[key("cl")])
    P.op(V, lambda e: e.tensor_scalar(out=cl[:, :, 0], in0=cl[:, :, 1], scalar1=-8.0, scalar2=None, op0=ALU.mult),
         reads=[key("cl")], writes=[key("cl")])
    P.op(V, lambda e: e.tensor_scalar(out=cl[:, :, 1], in0=cl[:, :, 1], scalar1=-16.0, scalar2=None, op0=ALU.mult),
         reads=[key("cl")], writes=[key("cl")])
    P.op(V, lambda e: e.tensor_scalar(out=gq8[:], in0=pv[:, 18:19], scalar1=0.125, scalar2=None, op0=ALU.mult),
         reads=[key("pv")], writes=[key("gq8")])

    def pvc(col):
        return pv[:, col:col + 1]

    if stop <= 0:
        return [key('gmix')]
    for i in range(nchunk):
        nTi = nT[i % 2]
        knT = key("nT", i % 2)
        qa = q_aug[i % 2]
        kqa = key("qaug", i % 2)
        c0 = i * 512
        for tl in range(4):
            tt = i * 4 + tl
            b = tt % 2
            x_t, s_t, n_t = xt[b], st[b], nb[b]
            kx, ks, kn = key("xt", b), key("st", b), key("nb")
            P.dma("sync", x_t[:], (d["xb_tile"](tt) if "xb_tile" in d else d["xb"][tt * 128:(tt + 1) * 128, :]), writes=[kx])
            P.op(SC, lambda e, x_t=x_t, s_t=s_t, n_t=n_t: e.activation(out=n_t[:], in_=x_t[:], func=AF.Square,
                                                              accum_out=s_t[:, 0:1]),
                 reads=[kx], writes=[kn, ks])
            P.op(SC, lambda e, s_t=s_t: e.activation(out=s_t[:, 1:2], in_=s_t[:, 0:1], func=AF.Ln, scale=1.0 / 1024,
                                                     bias=eps[:]),
                 reads=[ks, key("eps")], writes=[ks])
            P.op(SC, lambda e, s_t=s_t: e.activation(out=s_t[:, 2:3], in_=s_t[:, 1:2], func=AF.Exp, scale=-0.5),
                 reads=[ks], writes=[ks])
            P.op(V, lambda e, x_t=x_t, s_t=s_t, n_t=n_t: e.scalar_tensor_tensor(
                out=n_t[:], in0=x_t[:], scalar=s_t[:, 2:3], in1=gmix[:], op0=ALU.mult, op1=ALU.mult),
                reads=[kx, ks, key("gmix")], writes=[kn])
            for kc in range(8):
                P.op(T, lambda e, kc=kc, n_t=n_t: e.transpose(out=pT[:, kc, :], in_=n_t[:, kc * 128:(kc + 1) * 128],
                                                              identity=ident_b[:]),
                     reads=[kn, key("ident_b")], writes=[key("pT")])
            P.op(V, lambda e, tl=tl, nTi=nTi: e.tensor_copy(out=nTi[:, :, tl * 128:(tl + 1) * 128], in_=pT[:]),
                 reads=[key("pT")], writes=[knT + (tl,)])

        def proj(ps, kps, col0, ncol, npart_out=None):
            for kc in range(8):
                P.op(T, lambda e, kc=kc: e.matmul(ps[0:ncol, :], lhsT=wb[:, kc, col0:col0 + ncol], rhs=nTi[:, kc, :],
                                                  start=(kc == 0), stop=(kc == 7)),
                     reads=[key("wb"), knT], writes=[kps])

        if stop <= 1:
            continue
        st_ = {}

        def S1(c):
            zx_ps, kzx = next_pa()
            proj(zx_ps, kzx, c * 128, 128)
            P.op(SC, lambda e: e.copy(out=zxb[c][:, 3:515], in_=zx_ps[:]),
                 reads=[kzx], writes=[key("zxb", c, "main")])

        def S2(c):
            P.op(V, lambda e: e.tensor_scalar(out=xc[c][:], in0=zxb[c][:, 3:515], scalar1=pvc(3 * 2 + c),
                                              scalar2=pvc(8 + c), op0=ALU.mult, op1=ALU.add),
                 reads=[key("zxb", c), key("pv")], writes=[key("xc", c)])
            for dl in (1, 2, 3):
                P.op(V, lambda e, dl=dl: e.scalar_tensor_tensor(
                    out=xc[c][:], in0=zxb[c][:, 3 - dl:515 - dl], scalar=pvc((3 - dl) * 2 + c), in1=xc[c][:],
                    op0=ALU.mult, op1=ALU.add),
                    reads=[key("zxb", c), key("pv"), key("xc", c)], writes=[key("xc", c)])
            P.op(G, lambda e: e.tensor_copy(out=zxb[c][:, 0:3], in_=zxb[c][:, 512:515]),
                 reads=[key("zxb", c, "main")], writes=[key("zxb", c, "halo")])
            P.op(G, lambda e: e.tensor_copy(out=xcb[c][:], in_=xc[c][:]), reads=[key("xc", c)], writes=[key("xcb", c)])

        def S3(c):
            r_ps, kr = next_pa()
            P.op(T, lambda e: e.matmul(r_ps[:], lhsT=wr[:, c, :], rhs=xcb[c][:], start=True, stop=True),
                 reads=[key("wr"), key("xcb", c)], writes=[kr])
            i_ps, ki = next_pa()
            P.op(T, lambda e: e.matmul(i_ps[:], lhsT=wi[:, c, :], rhs=xcb[c][:], start=True, stop=True),
                 reads=[key("wi"), key("xcb", c)], writes=[ki])
            st_[c] = (r_ps, kr, i_ps, ki)

        def S4(c):
            r_ps, kr, i_ps, ki = st_[c]
            P.op(SC, lambda e: e.activation(out=rr[c][:], in_=r_ps[:], func=AF.Sigmoid, bias=pvc(10 + c)),
                 reads=[kr, key("pv")], writes=[key("rr", c)])
            P.op(SC, lambda e: e.activation(out=ig[c][:], in_=i_ps[:], func=AF.Sigmoid, bias=pvc(12 + c)),
                 reads=[ki, key("pv")], writes=[key("ig", c)])

        def S5(c):
            P.op(SC, lambda e: e.activation(out=aa[c][:], in_=rr[c][:], func=AF.Exp, scale=cl[:, c, 0:1]),
                 reads=[key("rr", c), key("cl")], writes=[key("aa", c)])
            P.op(SC, lambda e: e.activation(out=a2[c][:], in_=rr[c][:], func=AF.Exp, scale=cl[:, c, 1:2]),
                 reads=[key("rr", c), key("cl")], writes=[key("a2", c)])
            P.op(G, lambda e: e.tensor_tensor(out=uu[c][:], in0=ig[c][:], in1=xc[c][:], op=ALU.mult),
                 reads=[key("ig", c), key("xc", c)], writes=[key("uu", c)])

        def S5b(c):
            P.op(SC, lambda e: e.activation(out=sq_[c][:], in_=a2[c][:], func=AF.Ln, scale=-1.0, bias=one1[:]),
                 reads=[key("a2", c), key("one1")], writes=[key("sq_", c)])
            P.op(SC, lambda e: e.activation(out=sq_[c][:], in_=sq_[c][:], func=AF.Exp, scale=0.5),
                 reads=[key("sq_", c)], writes=[key("sq_", c)])

        def S6(c):
            P.op(V, lambda e: e.tensor_tensor(out=uu[c][:], in0=uu[c][:], in1=sq_[c][:], op=ALU.mult),
                 reads=[key("uu", c), key("sq_", c)], writes=[key("uu", c)])
            P.op(V, lambda e: e.tensor_tensor_scan(out=hh[c][:], data0=aa[c][:], data1=uu[c][:],
                                                   initial=hst[:, c:c + 1], op0=ALU.mult, op1=ALU.add),
                 reads=[key("aa", c), key("uu", c), key("hst", c)], writes=[key("hh", c)])
            P.op(G, lambda e: e.tensor_copy(out=hst[:, c:c + 1], in_=hh[c][:, 511:512]),
                 reads=[key("hh", c)], writes=[key("hst", c)])

        def S7(c):
            zg_ps, kzg = next_pa()
            proj(zg_ps, kzg, 256 + c * 128, 128)
            P.op(SC, lambda e: e.activation(out=gg[c][:], in_=zg_ps[:], func=AF.Gelu_apprx_tanh),
                 reads=[kzg], writes=[key("gg", c)])

        def S8(c):
            P.op(V, lambda e: e.tensor_tensor(out=yy[c][:], in0=hh[c][:], in1=gg[c][:], op=ALU.mult),
                 reads=[key("hh", c), key("gg", c)], writes=[key("yy", c)])
            P.op(G, lambda e: e.tensor_tensor(out=ysq[c][:], in0=yy[c][:], in1=yy[c][:], op=ALU.mult),
                 reads=[key("yy", c)], writes=[key("ysq", c)])
            P.op(SC, lambda e: e.activation(out=ygb[c][:], in_=yy[c][:], func=AF.Identity, scale=pvc(16 + c)),
                 reads=[key("yy", c), key("pv")], writes=[key("ygb", c)])
            P.dma("sync", d["yT"][c * 128:(c + 1) * 128, c0:c0 + 512], ygb[c][:], reads=[key("ygb", c)],
                  writes=[key("yT_out")])

        for stg in (S1, S2, S3, S7, S4, S5, S5b, S6, S8):
            for c in range(2):
                stg(c)

        if stop <= 2:
            continue
        pairs = [(which, hp) for which in range(2) for hp in range(2)]
        zs = {}

        def qk_proj(n):
            which, hp = pairs[n]
            z_ps, kz = next_pa()
            proj(z_ps, kz, 512 + which * 256 + hp * 128, 128)
            zs[n] = (z_ps, kz)

        def qk_norm(n):
            which, hp = pairs[n]
            z_ps, kz = zs[n]
            par = n % 2
            P.op(SC, lambda e: e.activation(out=qsq[par][:], in_=z_ps[:], func=AF.Square),
                 reads=[kz], writes=[key("qsq", par)])
            s_ps, ks_ = next_pa()
            P.op(T, lambda e: e.matmul(s_ps[:], lhsT=ones_bd[:], rhs=qsq[par][:], start=True, stop=True),
                 reads=[key("ones_bd"), key("qsq", par)], writes=[ks_])
            P.op(SC, lambda e: e.activation(out=qlr[par][:], in_=s_ps[:], func=AF.Ln, scale=1.0 / 64, bias=eps[:]),
                 reads=[ks_, key("eps")], writes=[key("qlr", par)])
            P.op(SC, lambda e: e.activation(out=qrs[par][:], in_=qlr[par][:], func=AF.Exp, scale=-0.5),
                 reads=[key("qlr", par)], writes=[key("qrs", par)])
            gcol = gq8[:, 0:1] if which == 0 else pv[:, 19:20]
            kg = key("gq8") if which == 0 else key("pv")
            tmp = Pt[n % 4]
            ktmp = key("Pt", n % 4)
            hlA, hlB = 2 * hp, 2 * hp + 1
            if which == 0:
                dstA, kdA = qa[0:64, hlA, :], kqa + ("q", hlA)
                dstB, kdB = qa[0:64, hlB, :], kqa + ("q", hlB)
            else:
                dstA, kdA = k_aug[0:64, hlA, c0:c0 + 512], key("kaug", "k", hlA, i)
                dstB, kdB = k_aug[0:64, hlB, c0:c0 + 512], key("kaug", "k", hlB, i)
            P.op(V, lambda e: e.scalar_tensor_tensor(
                out=dstA, in0=qrs[par][0:64, :], scalar=gcol[0:64, :], in1=z_ps[0:64, :], op0=ALU.mult, op1=ALU.mult),
                reads=[key("qrs", par), kg, kz], writes=[kdA])
            P.op(V, lambda e: e.scalar_tensor_tensor(
                out=tmp[64:128, :], in0=qrs[par][64:128, :], scalar=gcol[64:128, :], in1=z_ps[64:128, :],
                op0=ALU.mult, op1=ALU.mult),
                reads=[key("qrs", par), kg, kz], writes=[ktmp])
            P.dma("sync", dstB, tmp[64:128, :], reads=[ktmp], writes=[kdB])

        qk_proj(0)
        for n in range(4):
            if n + 1 < 4:
                qk_proj(n + 1)
            qk_norm(n)

        for tl in range(4):
            j = i * 4 + tl
            v_ps, kv = next_pa()
            for kc in range(8):
                P.op(T, lambda e, kc=kc, tl=tl, v_ps=v_ps: e.matmul(
                    v_ps[:, 0:256], lhsT=nTi[:, kc, tl * 128:(tl + 1) * 128], rhs=wb[:, kc, 1024:1280],
                    start=(kc == 0), stop=(kc == 7)),
                    reads=[key("wb"), knT], writes=[kv])
            P.op(SC, lambda e, j=j, v_ps=v_ps: e.copy(out=Vp[:, j, :, 0:64],
                                                      in_=v_ps[:, 0:256].rearrange("p (h d) -> p h d", h=4)),
                 reads=[kv], writes=[key("Vp", "v", j)])
            for kc in range(8):
                P.op(T, lambda e, kc=kc, tl=tl: e.matmul(
                    misc[:, 0:4], lhsT=nTi[:, kc, tl * 128:(tl + 1) * 128], rhs=wb[:, kc, 1280:1284],
                    start=(kc == 0), stop=(kc == 7)),
                    reads=[key("wb"), knT], writes=[key("misc")])
            P.op(V, lambda e: e.tensor_tensor(out=ft[:], in0=misc[:, 0:4], in1=bfb[:], op=ALU.add),
                 reads=[key("misc"), key("bfb")], writes=[key("ft")])
            P.op(SC, lambda e: e.activation(out=fe[:], in_=ft[:], func=AF.Exp, scale=-1.0),
                 reads=[key("ft")], writes=[key("fe")])
            P.op(SC, lambda e: e.activation(out=nl, in_=fe[:], func=AF.Ln, bias=one1[:]),
                 reads=[key("fe"), key("one1")], writes=[key("nl")])
            if stop <= 3.4:
                continue
            P.op(T, lambda e: e.matmul(misc[:, 8:12], lhsT=tri_f[:], rhs=nl, start=True, stop=True),
                 reads=[key("tri_f"), key("nl")], writes=[key("misc")])
            P.op(T, lambda e: e.matmul(misc[:, 16:20], lhsT=ones_f[:], rhs=nl, start=True, stop=True),
                 reads=[key("ones_f"), key("nl")], writes=[key("misc")])
            P.op(V, lambda e, j=j: e.tensor_tensor(out=nc_tok[:, j, :], in0=misc[:, 8:12], in1=carry[:], op=ALU.add),
                 reads=[key("misc"), key("carry")], writes=[key("nc_tok", j)])
            P.op(V, lambda e: e.tensor_tensor(out=carry[:], in0=misc[:, 16:20], in1=carry[:], op=ALU.add),
                 reads=[key("misc"), key("carry")], writes=[key("carry")])
            P.op(V, lambda e, tl=tl: e.tensor_copy(out=nlb[:, tl, :, 64], in_=nl), reads=[key("nl")], writes=[key("nlb", tl)])
        cr = cref[i % 2]
        kcr = key("cref", i % 2)
        P.op(V, lambda e, cr=cr: e.tensor_copy(out=cr[:], in_=carry[:]), reads=[key("carry")], writes=[kcr])
        for hl in range(4):
            for a_ in range(4):
                for a2_ in range(a_, 4):
                    P.op(T, lambda e, hl=hl, a_=a_, a2_=a2_: e.matmul(
                        misc[0:65, 384:512],
                        lhsT=nlb[:, a2_, hl, :], rhs=(low_b[:] if a2_ == a_ else ones_b[:]),
                        start=(a2_ == a_), stop=(a2_ == 3)),
                        reads=[key("nlb"), key("low_b"), key("ones_b")], writes=[key("misc")])
                P.op(V, lambda e, hl=hl, a_=a_: e.tensor_copy(out=qa[64:65, hl, a_ * 128:(a_ + 1) * 128],
                                                             in_=misc[64:65, 384:512]),
                     reads=[key("misc")], writes=[kqa + ("aug", hl, a_)])
        if stop <= 3.9:
            continue
        bt = bias_t[i % 2]
        kbt = key("bias", i % 2)
        nj = 4 * i + 4
        for hl in range(4):
            P.op(V, lambda e, hl=hl, bt=bt, cr=cr, nj=nj: e.tensor_scalar(
                out=bt[:, 0:nj, hl], in0=nc_tok[:, 0:nj, hl], scalar1=cr[:, hl:hl + 1], scalar2=None,
                op0=ALU.subtract),
                reads=[key("nc_tok"), kcr], writes=[kbt])

        if stop <= 4:
            continue
        for hl in range(4):
            def emit_S(j, hl=hl):
                m = j - 4 * i
                off = 128 * m if m > 0 else 0
                sp, ksp = s_pool[j % 4]
                pt = Pt[j % 4]
                kpt = key("Pt", j % 4)
                P.op(T, lambda e: e.matmul(
                    sp[:, off:512], lhsT=k_aug[0:65, hl, j * 128:(j + 1) * 128], rhs=qa[0:65, hl, off:512],
                    start=True, stop=True),
                    reads=[key("kaug"), kqa], writes=[ksp])
                P.op(SC, lambda e: e.activation(
                    out=pt[:, off:512], in_=sp[:, off:512], func=AF.Exp, bias=bt[:, j, hl:hl + 1]),
                    reads=[ksp, kbt], writes=[kpt])
                if m >= 0:
                    P.op(G, lambda e: e.tensor_tensor(out=pt[:, off:off + 128], in0=pt[:, off:off + 128],
                                                      in1=tri_b[:], op=ALU.mult),
                         reads=[kpt, key("tri_b")], writes=[kpt])

            def emit_O(j, hl=hl):
                m = j - 4 * i
                off = 128 * m if m > 0 else 0
                pt = Pt[j % 4]
                kpt = key("Pt", j % 4)
                P.op(T, lambda e: e.matmul(
                    ops_[:, off:512], lhsT=Vp[:, j, hl, :], rhs=pt[:, off:512], start=(j == 0), stop=(j == nj - 1)),
                    reads=[key("Vp"), kpt], writes=[key("ops")])

            for j0 in range(min(3, nj)):
                emit_S(j0)
            for j in range(nj):
                if j + 3 < nj:
                    emit_S(j + 3)
                emit_O(j)
            P.op(SC, lambda e: e.copy(out=den_sb[64:128, :], in_=ops_[64:128, :]), reads=[key("ops")],
                 writes=[key("den_sb")])
            P.op(V, lambda e: e.tensor_copy(out=dhi[64:128, :], in_=den_sb[64:128, :]), reads=[key("den_sb")],
                 writes=[key("dhi")])
            P.op(V, lambda e: e.tensor_tensor(out=dlo[64:128, :], in0=den_sb[64:128, :], in1=dhi[64:128, :],
                                              op=ALU.subtract),
                 reads=[key("den_sb"), key("dhi")], writes=[key("dlo")])
            P.op(T, lambda e: e.matmul(dps[0:64, :], lhsT=shb[:], rhs=dhi[:], start=True, stop=False),
                 reads=[key("shb"), key("dhi")], writes=[key("dps")])
            P.op(T, lambda e: e.matmul(dps[0:64, :], lhsT=shb[:], rhs=dlo[:], start=False, stop=True),
                 reads=[key("shb"), key("dlo")], writes=[key("dps")])
            lden, rinv = qlr[hl % 2][0:64, :], qrs[hl % 2][0:64, :]
            P.op(SC, lambda e: e.activation(out=lden, in_=dps[0:64, :], func=AF.Ln),
                 reads=[key("dps")], writes=[key("qlr", hl % 2)])
            P.op(SC, lambda e: e.activation(out=rinv, in_=lden, func=AF.Exp, scale=-1.0),
                 reads=[key("qlr", hl % 2)], writes=[key("qrs", hl % 2)])
            P.op(V, lambda e: e.tensor_tensor(out=yat[:], in0=ops_[0:64, :], in1=rinv, op=ALU.mult),
                 reads=[key("ops"), key("qrs", hl % 2)], writes=[key("yat")])
            P.op(G, lambda e, hl=hl: e.tensor_tensor(out=asq[hl][:], in0=yat[:], in1=yat[:], op=ALU.mult),
                 reads=[key("yat")], writes=[key("asq", hl)])
            P.op(SC, lambda e, hl=hl: e.activation(out=agb[hl % 2][:], in_=yat[:], func=AF.Copy,
                                                   scale=pv[0:64, 20 + hl:21 + hl]),
                 reads=[key("yat"), key("pv")], writes=[key("agb", hl % 2)])
            P.dma("sync", d["yT"][256 + hl * 64:256 + (hl + 1) * 64, c0:c0 + 512], agb[hl % 2][:],
                  reads=[key("agb", hl % 2)], writes=[key("yT_out")])
        if stop <= 5:
            continue
        for tl in range(4):
            for c in range(2):
                P.op(T, lambda e, tl=tl, c=c: e.matmul(misc[:, 256 + tl * 2:257 + tl * 2],
                                                       lhsT=ysq[c][:, tl * 128:(tl + 1) * 128], rhs=ones_b[:, 0:1],
                                                       start=(c == 0), stop=(c == 1)),
                     reads=[key("ysq", c), key("ones_b")], writes=[key("misc")])
            for hl in range(4):
                P.op(T, lambda e, tl=tl, hl=hl: e.matmul(misc[:, 257 + tl * 2:258 + tl * 2],
                                                         lhsT=asq[hl][:, tl * 128:(tl + 1) * 128],
                                                         rhs=ones_b[0:64, 0:1], start=(hl == 0), stop=(hl == 3)),
                     reads=[key("asq", hl), key("ones_b")], writes=[key("misc")])
        P.op(V, lambda e: e.tensor_copy(out=sso[:], in_=misc[:, 256:264].rearrange("p (t k) -> p t k", k=2)),
             reads=[key("misc")], writes=[key("sso")])
        P.dma("sync", d["ss"][c0:c0 + 512, :].rearrange("(t p) k -> p t k", p=128), sso[:], reads=[key("sso")],
              writes=[key("ss_out")])
    return [key("yT_out"), key("ss_out")]


CAP = 512
NSLOT = 32 * CAP
TABW = 16


def token_phase(P, nc, d, NT=2048, pfx="t", nexp=32, gather=False, after_tile=None):
    def key(*a):
        return (pfx + "_" + str(a[0]),) + tuple(a[1:])

    sb = lambda shape, dt, name: P.sb(shape, dt, pfx + "_" + name)
    NTL = NT // 128
    NCH = NT // 512
    V, SC, G, T = "vector", "scalar", "gpsimd", "tensor"

    xres = sb([128, NTL, 1024], F32, "xres")
    nTt = sb([128, 8, 128], BF16, "nTt")
    XT1 = sb([128, 8, 512], BF16, "XT")
    XT = [XT1, XT1]
    Xg = [sb([128, 4, 1024], BF16, f"Xg{i}") for i in range(2)]
    ysb = [sb([128, 1024], BF16, f"ysb{i}") for i in range(2)]
    idxe = [sb([128, 4, TABW], I32, f"idxe{i}") for i in range(2)]
    zt = sb([128, 16, TABW], I32, "zt")
    tidt = sb([128, NTL, TABW], I32, "tidt")
    posi = sb([128, NTL, 2], I32, "posi")
    w12 = sb([128, NTL, 2], F32, "w12")
    cnt = sb([128, 32], F32, "cnt")
    eci = sb([128, 32], I32, "eci")
    ec = sb([128, 32], F32, "ec")
    dci = sb([128, 1], I32, "dci")
    dcol = sb([128, 1], F32, "dcol")
    Aoh = sb([128, 32], F32, "Aoh")
    Ab = sb([128, 32], BF16, "Ab")
    wfull = sb([128, 32], F32, "wfull")
    rank = sb([128, 32], F32, "rank")
    val = sb([128, 32], F32, "val")
    tmp32 = sb([128, 32], F32, "tmp32")
    m8v = sb([128, 8], F32, "m8v")
    posf = sb([128, 2], F32, "posf")
    negf = sb([128, 2], F32, "negf")
    triS = sb([128, 128], BF16, "triS")
    eg = [sb([128, 8, 512], BF16, f"eg{i}") for i in range(2)]
    eu = [sb([128, 8, 512], BF16, f"eu{i}") for i in range(2)]
    ed = [sb([128, 4, 1024], BF16, f"ed{i}") for i in range(2)]
    wbig = sb([128, 8, 1024], BF16, "wbig")
    wpp = sb([128, 2, 1024], BF16, "wpp")
    hT = sb([128, 4, 512], BF16, "hT")
    sgt = [sb([128, 512], F32, f"sgt{i}") for i in range(2)]
    bc1 = sb([128, 1024], F32, "bc1")
    bc2 = sb([128, 1024], F32, "bc2")
    w1 = sb([128, 1024], F32, "w1")
    w2 = sb([128, 1024], F32, "w2")
    nhi = sb([128, 1024], BF16, "nhi")
    nlo = sb([128, 1024], BF16, "nlo")
    nTlo = sb([128, 8, 128], BF16, "nTlo")
    yts = [sb([128, 2, 4, 128], BF16, f"yt{i}") for i in range(2)]
    pt_ = sb([128, 256], F32, "pt")
    pb = sb([128, 256], BF16, "pb")
    pTs = sb([128, 2, 128], BF16, "pTs")
    wrf = sb([128, 8, 36], F32, "wrf")
    wrh = sb([128, 8, 36], BF16, "wrh")
    wrl = sb([128, 8, 36], BF16, "wrl")
    brt = sb([128, 36], F32, "brt")
    ident_b = sb([128, 128], BF16, "ident_b")
    ones_b = sb([128, 128], BF16, "ones_b")
    eps = sb([128, 1], F32, "eps")
    ssts = [sb([128, 2, 2], F32, f"sst{i}") for i in range(4)]
    sm = sb([128, 16], F32, "sm")
    lgs = sb([128, 36], F32, "lgs")
    oh = sb([128, 4], F32, "oh")
    ge = sb([128, 4], F32, "ge")
    el8 = sb([128, 8], F32, "el8")
    m8 = sb([128, 8], F32, "m8")
    sel8 = sb([128, 8], F32, "sel8")
    ee8 = sb([128, 8], F32, "ee8")
    w8 = sb([128, 8], F32, "w8")
    A = [P.ps([128, 512], F32, pfx + f"_A{i}") for i in range(2)]
    B = [P.ps([128, 512], F32, pfx + f"_B{i}") for i in range(2)]
    Y = [P.ps([128, 512], F32, pfx + f"_Y{i}") for i in range(2)]
    pT = P.ps([128, 8, 128], BF16, pfx + "_pT")
    misc = P.ps([128, 512], F32, pfx + "_misc")

    if gather:
        idxy = sb([128, 32], I32, "idxy")
        idxs = sb([128, 2 * NTL], I32, "idxs")
        P.dma("sync", idxy[:], d["idx_y4"][:, :], writes=[key("idxy")])
        P.dma("sync", idxs[:], d["idx_s"][:, :], writes=[key("idxs")])
    P.dma("sync", bc1[:], d["ffn_norm"].partition_broadcast(128), writes=[key("bc1")])
    P.dma("sync", brt[:], d["brt"].partition_broadcast(128), writes=[key("brt")])
    P.dma("sync", wrf[:], d["wrt"].rearrange("(c p) n -> p c n", p=128), writes=[key("wrf")])
    P.dma("gpsimd", wbig[:], d["wout"].rearrange("(c p) n -> p c n", p=128), writes=[key("wbig")])
    P.dma("gpsimd", wpp[:], d["wpp"].rearrange("(c p) n -> p c n", p=128), writes=[key("wpp")])
    P.op(V, lambda e: e.memset(eps[:], EPS), writes=[key("eps")])
    P.op(V, lambda e: e.memset(ones_b[:], 1.0), writes=[key("ones_b")])
    P.op(G, lambda e: e.affine_select(out=ident_b[:], in_=ones_b[:], pattern=[[1, 128]], compare_op=ALU.is_equal,
                                      fill=0.0, base=0, channel_multiplier=-1),
         reads=[key("ones_b")], writes=[key("ident_b")])
    P.op(G, lambda e: e.affine_select(out=triS[:], in_=ones_b[:], pattern=[[1, 128]], compare_op=ALU.is_gt,
                                      fill=0.0, base=0, channel_multiplier=-1),
         reads=[key("ones_b")], writes=[key("triS")])
    P.op(G, lambda e: e.iota(tidt[:], pattern=[[128, NTL], [0, TABW]], base=0, channel_multiplier=1),
         writes=[key("tidt")])
    P.op(G, lambda e: e.iota(eci[:], pattern=[[CAP, 32]], base=1, channel_multiplier=0), writes=[key("eci")])
    P.op(G, lambda e: e.iota(dci[:], pattern=[[0, 1]], base=NSLOT + 1, channel_multiplier=1), writes=[key("dci")])
    P.op(V, lambda e: e.tensor_copy(out=ec[:], in_=eci[:]), reads=[key("eci")], writes=[key("ec")])
    P.op(V, lambda e: e.tensor_copy(out=dcol[:], in_=dci[:]), reads=[key("dci")], writes=[key("dcol")])
    P.op(V, lambda e: e.memset(cnt[:], 0.0), writes=[key("cnt")])
    P.op(V, lambda e: e.memset(zt[:], 0), writes=[key("zt")])
    P.op(V, lambda e: e.memset(nlo[:], 0.0), writes=[key("nlo")])
    tabv = d["tab"].rearrange("(p j) w -> p j w", p=128)
    for j8 in range(8):
        P.dma("sync", tabv[:, j8 * 16:(j8 + 1) * 16, :], zt[:], reads=[key("zt")], writes=[("tab", "z", j8)])
    P.dma("sync", tabv[:, 128:129, :], zt[:, 0:1, :], reads=[key("zt")], writes=[("tab", "z", 8)])
    P.dma("sync", d["ytab"][NSLOT:NSLOT + 128, :], nlo[:], reads=[key("nlo")], writes=[("ytab", "dump")])
    P.op(V, lambda e: e.tensor_copy(out=wrh[:], in_=wrf[:]), reads=[key("wrf")], writes=[key("wrh")])
    P.op(V, lambda e: e.tensor_tensor(out=wrl[:], in0=wrf[:], in1=wrh[:], op=ALU.subtract),
         reads=[key("wrf"), key("wrh")], writes=[key("wrl")])

    def load_expert(e_):
        b = e_ % 2
        P.dma("gpsimd", eg[b][:], d["weg"][e_].rearrange("(c p) n -> p c n", p=128), writes=[key("eg", b)])
        P.dma("gpsimd", eu[b][:], d["weu"][e_].rearrange("(c p) n -> p c n", p=128), writes=[key("eu", b)])
        P.dma("gpsimd", ed[b][:], d["wed"][e_].rearrange("(c p) n -> p c n", p=128), writes=[key("ed", b)])

    def rms(x_ap, kx, gt, kg, out_f32=None):
        P.op(SC, lambda e: e.activation(out=nlo[:], in_=x_ap, func=AF.Square, accum_out=sm[:, 0:1]),
             reads=[kx], writes=[key("nlo"), key("sm", 0)])
        P.op(SC, lambda e: e.activation(out=sm[:, 1:2], in_=sm[:, 0:1], func=AF.Ln, scale=1.0 / 1024, bias=eps[:]),
             reads=[key("sm", 0), key("eps")], writes=[key("sm", 1)])
        P.op(SC, lambda e: e.activation(out=sm[:, 2:3], in_=sm[:, 1:2], func=AF.Exp, scale=-0.5),
             reads=[key("sm", 1)], writes=[key("sm", 2)])
        P.op(V, lambda e: e.scalar_tensor_tensor(out=w1[:], in0=x_ap, scalar=sm[:, 2:3], in1=gt[:], op0=ALU.mult,
                                                 op1=ALU.mult),
             reads=[kx, key("sm", 2), kg], writes=[key("w1")])

    def transpose8(src, ksrc, dst_ap, kdst):
        for kc in range(8):
            P.op(T, lambda e, kc=kc: e.transpose(out=pT[:, kc, :], in_=src[:, kc * 128:(kc + 1) * 128],
                                                 identity=ident_b[:]),
                 reads=[ksrc, key("ident_b")], writes=[key("pT")])
        P.op(V, lambda e: e.tensor_copy(out=dst_ap, in_=pT[:]), reads=[key("pT")], writes=[kdst])

    def loads(tt):
        t0 = tt * 128
        yt = yts[tt % 2]
        sst = ssts[tt % 4]
        kyt = key("yt%d" % (tt % 2))
        ksst = key("sst%d" % (tt % 4))
        P.dma("sync", xres[:, tt, :], d["x"][t0:t0 + 128, :], writes=[key("xres", tt)])
        if gather:
            for g in range(2):
                cols = g * NTL + tt
                P.op(G, lambda e, g=g, cols=cols: e.indirect_dma_start(
                    out=sst[:, g, :], out_offset=None, in_=d["ssg"][:, :],
                    in_offset=bass.IndirectOffsetOnAxis(ap=idxs[:, cols:cols + 1], axis=0)),
                    reads=[key("idxs")], writes=[ksst + (g,)], dma=True)
        else:
            for g in range(2):
                P.dma("sync", yt[:, g, :, :], d["yT"][g, :, t0:t0 + 128].rearrange("(c p) t -> p c t", p=128),
                      writes=[kyt + (g,)])
            P.dma("sync", sst[:], d["ss"][:, t0:t0 + 128, :].rearrange("g p k -> p g k"), writes=[ksst])

    def ytg_view(grp):
        return Xg[grp % 2][:].rearrange("p s (a t) -> p (s a) t", a=2)

    def group_gathers(grp):
        v = ytg_view(grp)
        for q in range(8):
            col = q * 4 + grp
            P.op(G, lambda e, q=q, col=col: e.indirect_dma_start(
                out=v[:, q, :], out_offset=None, in_=d["yTg4"][:, :],
                in_offset=bass.IndirectOffsetOnAxis(ap=idxy[:, col:col + 1], axis=0)),
                reads=[key("idxy")], writes=[key("Xg", grp % 2, q // 2, q % 2)], dma=True)

    load_expert(0)
    if gather:
        group_gathers(0)

    okey = key
    SCR = ("sm", "w1", "nhi", "nlo", "nTt", "nTlo", "lgs", "ge", "oh", "el8", "m8", "sel8", "ee8", "w8", "wfull",
           "Aoh", "Ab", "rank", "tmp32", "val", "m8v", "posf", "negf")
    small = dict(lgs=([128, 36], F32), ge=([128, 4], F32), oh=([128, 4], F32), el8=([128, 8], F32), m8=([128, 8], F32),
                 sel8=([128, 8], F32), ee8=([128, 8], F32), w8=([128, 8], F32), wfull=([128, 32], F32),
                 Aoh=([128, 32], F32), Ab=([128, 32], BF16), rank=([128, 32], F32), tmp32=([128, 32], F32),
                 val=([128, 32], F32), m8v=([128, 8], F32), posf=([128, 2], F32), negf=([128, 2], F32),
                 sm=([128, 16], F32))
    hTb = hT[:].rearrange("p a b -> p (a b)")
    t1s = [dict(sm=sm, w1=w1, nhi=nhi, nlo=nlo, nTt=nTt, nTlo=nTlo, lgs=lgs, ge=ge, oh=oh, el8=el8, m8=m8, sel8=sel8,
                ee8=ee8, w8=w8, wfull=wfull, Aoh=Aoh, Ab=Ab, rank=rank, tmp32=tmp32, val=val, m8v=m8v, posf=posf,
                negf=negf),
           dict(w1=XT1[:].rearrange("p k t -> p (k t)").bitcast(F32)[:, 0:1024], nhi=ysb[0][:], nlo=ysb[1][:],
                nTt=hTb[:, 0:1024].rearrange("p (k t) -> p k t", k=8),
                nTlo=hTb[:, 1024:2048].rearrange("p (k t) -> p k t", k=8))]
    for nm_, (shp_, dt_) in small.items():
        t1s[1][nm_] = sb(shp_, dt_, nm_ + "_b")

    def t1_tile(tt):
        t0 = tt * 128
        par = tt % 2
        TT = t1s[par]
        sm, w1, nhi, nlo, nTt, nTlo = TT["sm"], TT["w1"], TT["nhi"], TT["nlo"], TT["nTt"], TT["nTlo"]
        lgs, ge, oh, el8, m8, sel8, ee8, w8 = (TT[n] for n in ("lgs", "ge", "oh", "el8", "m8", "sel8", "ee8", "w8"))
        wfull, Aoh, Ab, rank, tmp32, val, m8v, posf, negf = (TT[n] for n in ("wfull", "Aoh", "Ab", "rank", "tmp32",
                                                                               "val", "m8v", "posf", "negf"))
        PSs = [(A, "A"), (B, "B")] if par == 0 else [(Y, "Y"), (Y, "Y")]

        def key(*a):
            if par == 0 or a[0] not in SCR:
                return okey(*a)
            big = {"w1": ("XT",), "nhi": ("ysb", 0), "nlo": ("ysb", 1), "nTt": ("hT",), "nTlo": ("hT",)}
            if a[0] in big:
                return okey(*big[a[0]])
            return okey(a[0] + "_b", *a[1:])

        def transpose8(src, ksrc, dst_ap, kdst):
            P.hold()
            for kc in range(8):
                P.op(T, lambda e, kc=kc: e.transpose(out=pT[:, kc, :], in_=src[:, kc * 128:(kc + 1) * 128],
                                                     identity=ident_b[:]),
                     reads=[ksrc, key("ident_b")], writes=[key("pT")])
            P.op(V, lambda e: e.tensor_copy(out=dst_ap, in_=pT[:]), reads=[key("pT")], writes=[kdst])
            P.release()
        t0 = tt * 128
        kxr = key("xres", tt)
        yt = yts[tt % 2]
        sst = ssts[tt % 4]
        kyt = key("yt%d" % (tt % 2))
        ksst = key("sst%d" % (tt % 4))
        P.op(V, lambda e: e.tensor_tensor(out=sm[:, 4:6], in0=sst[:, 0, :], in1=sst[:, 1, :], op=ALU.add),
             reads=[ksst], writes=[key("sm", 4)])
        P.op(SC, lambda e: e.activation(out=sm[:, 6:8], in_=sm[:, 4:6], func=AF.Ln, scale=1.0 / 512, bias=eps[:]),
             reads=[key("sm", 4), key("eps")], writes=[key("sm", 6)])
        P.op(SC, lambda e: e.activation(out=sm[:, 8:10], in_=sm[:, 6:8], func=AF.Exp, scale=-0.5),
             reads=[key("sm", 6)], writes=[key("sm", 8)])
        for grp in range(2):
            PS, kPS = PSs[grp]
            for half in range(2):
                n_ = 0
                for g in range(2):
                    for c in range(2):
                        row0 = grp * 512 + g * 256 + c * 128
                        kc = row0 // 128
                        if gather:
                            q_ = g * 4 + grp * 2 + c
                            lhs = ytg_view(tt // 4)[:, q_, (tt % 4) * 128:(tt % 4 + 1) * 128]
                            klhs = okey("Xg", (tt // 4) % 2, q_ // 2, q_ % 2)
                        else:
                            lhs = yt[:, g, grp * 2 + c, :]
                            klhs = kyt + (g,)
                        P.op(T, lambda e, lhs=lhs, kc=kc, half=half, PS=PS, n_=n_: e.matmul(
                            PS[half][:], lhsT=lhs, rhs=wbig[:, kc, half * 512:(half + 1) * 512],
                            start=(n_ == 0), stop=(n_ == 3)),
                            reads=[klhs, okey("wbig")], writes=[okey(kPS, half)])
                        n_ += 1
            for half in range(2):
                P.op(V, lambda e, grp=grp, half=half, PS=PS: e.scalar_tensor_tensor(
                    out=xres[:, tt, half * 512:(half + 1) * 512], in0=PS[half][:], scalar=sm[:, 8 + grp:9 + grp],
                    in1=xres[:, tt, half * 512:(half + 1) * 512], op0=ALU.mult, op1=ALU.add),
                    reads=[okey(kPS, half), key("sm", 8), kxr], writes=[kxr])
        P.op(SC, lambda e: e.activation(out=nlo[:], in_=xres[:, tt, :], func=AF.Square, accum_out=sm[:, 0:1]),
             reads=[kxr], writes=[key("nlo"), key("sm", 0)])
        P.op(SC, lambda e: e.activation(out=sm[:, 1:2], in_=sm[:, 0:1], func=AF.Ln, scale=1.0 / 1024, bias=eps[:]),
             reads=[key("sm", 0), key("eps")], writes=[key("sm", 1)])
        P.op(SC, lambda e: e.activation(out=sm[:, 2:3], in_=sm[:, 1:2], func=AF.Exp, scale=-0.5),
             reads=[key("sm", 1)], writes=[key("sm", 2)])
        P.op(V, lambda e: e.scalar_tensor_tensor(out=w1[:], in0=xres[:, tt, :], scalar=sm[:, 2:3], in1=bc1[:],
                                                 op0=ALU.mult, op1=ALU.mult),
             reads=[kxr, key("sm", 2), key("bc1")], writes=[key("w1")])
        P.op(SC, lambda e: e.copy(out=nhi[:], in_=w1[:]), reads=[key("w1")], writes=[key("nhi")])
        P.op(V, lambda e: e.tensor_tensor(out=nlo[:], in0=w1[:], in1=nhi[:], op=ALU.subtract),
             reads=[key("w1"), key("nhi")], writes=[key("nlo")])
        P.dma("sync", d["n2tab"][t0:t0 + 128, :], nhi[:], reads=[key("nhi")], writes=[("n2tab", tt)])
        transpose8(nhi, key("nhi"), nTt[:], key("nTt"))
        transpose8(nlo, key("nlo"), nTlo[:], key("nTlo"))
        P.hold()
        n_ = 0
        for (a_, ka, w_, kw) in ((None, None, wrh, "wrh"), (nTlo, "nTlo", wrh, "wrh"), (None, None, wrl, "wrl")):
            for kc in range(8):
                lhs = nTt[:, kc, :] if a_ is None else a_[:, kc, :]
                P.op(T, lambda e, lhs=lhs, w_=w_, kc=kc, n_=n_: e.matmul(misc[:, 0:36], lhsT=lhs, rhs=w_[:, kc, :],
                                                                         start=(n_ == 0), stop=(n_ == 23)),
                     reads=[key("nTt"), key("nTlo"), key(kw)], writes=[key("misc")])
                n_ += 1
        P.op(V, lambda e: e.tensor_tensor(out=lgs[:], in0=misc[:, 0:36], in1=brt[:], op=ALU.add),
             reads=[key("misc"), key("brt")], writes=[key("lgs")])
        P.release()
        P.op(V, lambda e: e.tensor_reduce(out=sm[:, 10:11], in_=lgs[:, 0:4], axis=AX.X, op=ALU.max),
             reads=[key("lgs")], writes=[key("sm", 10)])
        P.op(V, lambda e: e.tensor_scalar(out=sm[:, 11:12], in0=sm[:, 10:11], scalar1=-1.0, scalar2=None, op0=ALU.mult),
             reads=[key("sm", 10)], writes=[key("sm", 11)])
        P.op(SC, lambda e: e.activation(out=ge[:], in_=lgs[:, 0:4], func=AF.Exp, bias=sm[:, 11:12],
                                        accum_out=sm[:, 12:13]),
             reads=[key("lgs"), key("sm", 11)], writes=[key("ge"), key("sm", 12)])
        P.op(V, lambda e: e.reciprocal(out=sm[:, 13:14], in_=sm[:, 12:13]), reads=[key("sm", 12)],
             writes=[key("sm", 13)])
        P.op(V, lambda e: e.tensor_scalar(out=oh[:], in0=lgs[:, 0:4], scalar1=sm[:, 10:11], scalar2=None,
                                          op0=ALU.is_equal),
             reads=[key("lgs"), key("sm", 10)], writes=[key("oh")])
        P.op(V, lambda e: e.tensor_scalar(out=el8[:], in0=lgs[:, 4:12], scalar1=oh[:, 0:1], scalar2=None, op0=ALU.mult),
             reads=[key("lgs"), key("oh")], writes=[key("el8")])
        for g in range(1, 4):
            P.op(V, lambda e, g=g: e.scalar_tensor_tensor(out=el8[:], in0=lgs[:, 4 + 8 * g:12 + 8 * g],
                                                          scalar=oh[:, g:g + 1], in1=el8[:], op0=ALU.mult, op1=ALU.add),
                 reads=[key("lgs"), key("oh"), key("el8")], writes=[key("el8")])
        P.op(V, lambda e: e.max(out=m8[:], in_=el8[:]), reads=[key("el8")], writes=[key("m8")])
        P.op(V, lambda e: e.tensor_scalar(out=sel8[:], in0=el8[:], scalar1=m8[:, 1:2], scalar2=None, op0=ALU.is_ge),
             reads=[key("el8"), key("m8")], writes=[key("sel8")])
        P.op(V, lambda e: e.tensor_scalar(out=sm[:, 14:15], in0=m8[:, 0:1], scalar1=-1.0, scalar2=None, op0=ALU.mult),
             reads=[key("m8")], writes=[key("sm", 14)])
        P.op(SC, lambda e: e.activation(out=ee8[:], in_=el8[:], func=AF.Exp, bias=sm[:, 14:15]),
             reads=[key("el8"), key("sm", 14)], writes=[key("ee8")])
        P.op(V, lambda e: e.tensor_tensor(out=ee8[:], in0=ee8[:], in1=sel8[:], op=ALU.mult),
             reads=[key("ee8"), key("sel8")], writes=[key("ee8")])
        P.op(V, lambda e: e.tensor_reduce(out=sm[:, 15:16], in_=ee8[:], axis=AX.X, op=ALU.add),
             reads=[key("ee8")], writes=[key("sm", 15)])
        P.op(V, lambda e: e.reciprocal(out=sm[:, 15:16], in_=sm[:, 15:16]), reads=[key("sm", 15)],
             writes=[key("sm", 15)])
        P.op(V, lambda e: e.tensor_tensor(out=sm[:, 15:16], in0=sm[:, 15:16], in1=sm[:, 13:14], op=ALU.mult),
             reads=[key("sm", 15), key("sm", 13)], writes=[key("sm", 15)])
        P.op(V, lambda e: e.tensor_scalar(out=w8[:], in0=ee8[:], scalar1=sm[:, 15:16], scalar2=None, op0=ALU.mult),
             reads=[key("ee8"), key("sm", 15)], writes=[key("w8")])
        for g in range(4):
            P.op(V, lambda e, g=g: e.tensor_scalar(out=wfull[:, 8 * g:8 * g + 8], in0=w8[:], scalar1=oh[:, g:g + 1],
                                                   scalar2=None, op0=ALU.mult),
                 reads=[key("w8"), key("oh")], writes=[key("wfull", g)])
            P.op(V, lambda e, g=g: e.tensor_scalar(out=Aoh[:, 8 * g:8 * g + 8], in0=sel8[:], scalar1=oh[:, g:g + 1],
                                                   scalar2=None, op0=ALU.mult),
                 reads=[key("sel8"), key("oh")], writes=[key("Aoh", g)])
        P.op(G, lambda e: e.tensor_copy(out=Ab[:], in_=Aoh[:]), reads=[key("Aoh")], writes=[key("Ab")])
        P.hold()
        P.op(T, lambda e: e.matmul(misc[:, 64:96], lhsT=triS[:], rhs=Ab[:], start=True, stop=True),
             reads=[key("triS"), key("Ab")], writes=[key("misc")])
        P.op(T, lambda e: e.matmul(misc[:, 96:128], lhsT=ones_b[:], rhs=Ab[:], start=True, stop=True),
             reads=[key("ones_b"), key("Ab")], writes=[key("misc")])
        P.op(V, lambda e: e.tensor_tensor(out=rank[:], in0=misc[:, 64:96], in1=cnt[:], op=ALU.add),
             reads=[key("misc"), key("cnt")], writes=[key("rank")])
        P.op(V, lambda e: e.tensor_tensor(out=cnt[:], in0=misc[:, 96:128], in1=cnt[:], op=ALU.add),
             reads=[key("misc"), key("cnt")], writes=[key("cnt")])
        P.release()
        P.op(V, lambda e: e.tensor_scalar(out=tmp32[:], in0=rank[:], scalar1=float(CAP), scalar2=None, op0=ALU.is_lt),
             reads=[key("rank")], writes=[key("tmp32")])
        P.op(V, lambda e: e.tensor_tensor(out=tmp32[:], in0=tmp32[:], in1=Aoh[:], op=ALU.mult),
             reads=[key("tmp32"), key("Aoh")], writes=[key("tmp32")])
        P.op(V, lambda e: e.tensor_tensor(out=val[:], in0=rank[:], in1=ec[:], op=ALU.add),
             reads=[key("rank"), key("ec")], writes=[key("val")])
        P.op(V, lambda e: e.tensor_tensor(out=val[:], in0=val[:], in1=tmp32[:], op=ALU.mult),
             reads=[key("val"), key("tmp32")], writes=[key("val")])
        P.op(V, lambda e: e.max(out=m8v[:], in_=val[:]), reads=[key("val")], writes=[key("m8v")])
        for k in range(2):
            P.op(V, lambda e, k=k: e.tensor_scalar(out=tmp32[:], in0=val[:], scalar1=m8v[:, k:k + 1], scalar2=None,
                                                   op0=ALU.is_equal),
                 reads=[key("val"), key("m8v")], writes=[key("tmp32")])
            P.op(V, lambda e: e.tensor_tensor(out=tmp32[:], in0=tmp32[:], in1=wfull[:], op=ALU.mult),
                 reads=[key("tmp32"), key("wfull")], writes=[key("tmp32")])
            P.op(V, lambda e, k=k, tt=tt: e.tensor_reduce(out=w12[:, tt, k:k + 1], in_=tmp32[:], axis=AX.X, op=ALU.add),
                 reads=[key("tmp32")], writes=[key("w12", tt, k)])
        P.op(V, lambda e: e.tensor_scalar(out=posf[:], in0=m8v[:, 0:2], scalar1=-1.0, scalar2=None, op0=ALU.add),
             reads=[key("m8v")], writes=[key("posf")])
        P.op(V, lambda e: e.tensor_scalar(out=negf[:], in0=posf[:], scalar1=0.0, scalar2=None, op0=ALU.is_lt),
             reads=[key("posf")], writes=[key("negf")])
        P.op(V, lambda e: e.scalar_tensor_tensor(out=posf[:], in0=negf[:], scalar=dcol[:, 0:1], in1=posf[:],
                                                 op0=ALU.mult, op1=ALU.add),
             reads=[key("negf"), key("dcol"), key("posf")], writes=[key("posf")])
        P.op(V, lambda e, tt=tt: e.tensor_copy(out=posi[:, tt, :], in_=posf[:]), reads=[key("posf")],
             writes=[key("posi", tt)])
        for k in range(2):
            P.op(G, lambda e, k=k, tt=tt: e.indirect_dma_start(
                out=d["tab"][:, :], out_offset=bass.IndirectOffsetOnAxis(ap=posi[:, tt, k:k + 1], axis=0),
                in_=tidt[:, tt, :], in_offset=None),
                reads=[key("posi", tt), key("tidt"), "tab"], writes=[("tab", "sc", tt, k)], dma=True)


    if gather:
        loads(0)
        loads(1)
    for tt in range(0, NTL, 2):
        if gather and tt % 4 == 0 and tt // 4 + 1 < NTL // 4:
            group_gathers(tt // 4 + 1)
        if gather:
            if tt + 2 < NTL:
                loads(tt + 2)
                loads(tt + 3)
        else:
            loads(tt)
            loads(tt + 1)
        P.capture(automark=True)
        t1_tile(tt)
        sa_ = P.end_capture()
        P.capture(automark=True)
        t1_tile(tt + 1)
        sb_ = P.end_capture()
        P.replay([sa_, sb_])

    def load_idx(e_):
        b_ = e_ % 2
        P.dma("sync", idxe[b_][:], d["tab"][e_ * CAP:(e_ + 1) * CAP, :].rearrange("(s p) w -> p s w", p=128),
              reads=["tab"], writes=[key("idxe", b_)])
        for s_ in range(4):
            P.op(G, lambda e, s_=s_, b_=b_: e.indirect_dma_start(
                out=Xg[b_][:, s_, :], out_offset=None, in_=d["n2tab"][:, :],
                in_offset=bass.IndirectOffsetOnAxis(ap=idxe[b_][:, s_, 0:1], axis=0)),
                reads=[key("idxe", b_), "n2tab"], writes=[key("Xg", b_, s_)], dma=True)

    pT2 = misc[:].bitcast(BF16).rearrange("p (k t) -> p k t", k=8)
    pTs_ = [(pT[:], key("pT")), (pT2, key("misc")),
            (Y[0][:].bitcast(BF16).rearrange("p (k t) -> p k t", k=8), key("Y", 0)),
            (Y[1][:].bitcast(BF16).rearrange("p (k t) -> p k t", k=8), key("Y", 1))]

    def emit_T(e_):
        b = e_ % 2
        for s_ in range(4):
            pt_ps, kpt_ps = pTs_[s_ % 4]
            for kc in range(8):
                P.op(T, lambda e, kc=kc, s_=s_, b=b, pt_ps=pt_ps: e.transpose(
                    out=pt_ps[:, kc, :], in_=Xg[b][:, s_, kc * 128:(kc + 1) * 128], identity=ident_b[:]),
                    reads=[key("Xg", b, s_), key("ident_b")], writes=[kpt_ps])
            if s_ % 2 == 0:
                P.op(V, lambda e, s_=s_, pt_ps=pt_ps: e.tensor_copy(out=XT1[:, :, s_ * 128:(s_ + 1) * 128], in_=pt_ps),
                     reads=[kpt_ps], writes=[key("XT", s_)])
            else:
                P.op(SC, lambda e, s_=s_, pt_ps=pt_ps: e.copy(out=XT1[:, :, s_ * 128:(s_ + 1) * 128], in_=pt_ps),
                     reads=[kpt_ps], writes=[key("XT", s_)])

    def emit_h(e_):
        b = e_ % 2
        for fc in range(4):
            for (W_, kw, PS, kp) in ((eg[b], key("eg", b), A[fc % 2], key("A", fc % 2)),
                                     (eu[b], key("eu", b), B[fc % 2], key("B", fc % 2))):
                for kc in range(8):
                    P.op(T, lambda e, W_=W_, PS=PS, kc=kc, fc=fc: e.matmul(
                        PS[:], lhsT=W_[:, kc, fc * 128:(fc + 1) * 128], rhs=XT1[:, kc, :],
                        start=(kc == 0), stop=(kc == 7)),
                        reads=[kw, key("XT")], writes=[kp])
            P.op(SC, lambda e, fc=fc: e.activation(out=sgt[fc % 2][:], in_=A[fc % 2][:], func=AF.Silu),
                 reads=[key("A", fc % 2)], writes=[key("sgt", fc % 2)])
            P.op(V, lambda e, fc=fc: e.tensor_tensor(out=hT[:, fc, :], in0=B[fc % 2][:], in1=sgt[fc % 2][:],
                                                     op=ALU.mult),
                 reads=[key("B", fc % 2), key("sgt", fc % 2)], writes=[key("hT", fc)])

    w1b = w1[:].bitcast(BF16)
    w2b = w2[:].bitcast(BF16)
    ysb_pool = [(ysb[0][:], key("ysb", 0)), (ysb[1][:], key("ysb", 1)),
                (w1b[:, 0:1024], key("w1", "a")), (w1b[:, 1024:2048], key("w1", "b")),
                (w2b[:, 0:1024], key("w2", 0)), (w2b[:, 1024:2048], key("w2", 1))]

    def emit_y(e_):
        b = e_ % 2
        for s_ in range(4):
            yb, kyb = ysb_pool[(e_ * 4 + s_) % len(ysb_pool)]
            YB, ynm = ((Y, "Y"), (A, "A"), (B, "B"))[s_ % 3]
            for half in range(2):
                for fc in range(4):
                    P.op(T, lambda e, fc=fc, s_=s_, half=half, b=b, YB=YB: e.matmul(
                        YB[half][:], lhsT=hT[:, fc, s_ * 128:(s_ + 1) * 128],
                        rhs=ed[b][:, fc, half * 512:(half + 1) * 512], start=(fc == 0), stop=(fc == 3)),
                        reads=[key("hT"), key("ed", b)], writes=[key(ynm, half)])
                if half == 0:
                    P.op(SC, lambda e, yb=yb, YB=YB: e.copy(out=yb[:, 0:512], in_=YB[0][:]), reads=[key(ynm, 0)],
                         writes=[kyb + (0,)])
                else:
                    P.op(V, lambda e, yb=yb, YB=YB: e.tensor_copy(out=yb[:, 512:1024], in_=YB[1][:]),
                         reads=[key(ynm, 1)], writes=[kyb + (1,)])
            r0 = e_ * CAP + s_ * 128
            P.dma("sync", d["ytab"][r0:r0 + 128, :], yb, reads=[kyb], writes=[("ytab", e_, s_)])

    load_idx(0)
    emit_T(0)
    for e_ in range(nexp):
        if e_ + 1 < nexp:
            load_idx(e_ + 1)
            load_expert(e_ + 1)
        emit_h(e_)
        if e_ + 1 < nexp:
            emit_T(e_ + 1)
        emit_y(e_)
    P.dma("sync", bc1[:], d["ple_norm"].partition_broadcast(128), writes=[key("bc1")])
    P.dma("sync", bc2[:], d["b_ple"].partition_broadcast(128), writes=[key("bc2")])
    P.dma("gpsimd", wbig[:], d["wpg"].rearrange("(c p) n -> p c n", p=128), writes=[key("wbig")])
    pTs2 = sb([128, 2, 128], BF16, "pTs2")
    w3 = XT1[:].rearrange("p k t -> p (k t)").bitcast(F32)[:, 0:1024]
    nT3 = [nTlo, nTt]
    knT3 = [key("nTlo"), key("nTt")]
    pTsl = [pTs, pTs2]
    kpTs = [key("pTs"), key("pTs2")]

    def gback(tt):
        for k in range(2):
            gb = Xg[tt % 2]
            kgb = key("Xg", tt % 2, k)
            P.op(G, lambda e, k=k, gb=gb: e.indirect_dma_start(
                out=gb[:, k, :], out_offset=None, in_=d["ytab"][:, :],
                in_offset=bass.IndirectOffsetOnAxis(ap=posi[:, tt, k:k + 1], axis=0)),
                reads=[key("posi", tt), "ytab"], writes=[kgb], dma=True)
            P.op(V, lambda e, k=k, gb=gb: e.scalar_tensor_tensor(
                out=xres[:, tt, :], in0=gb[:, k, :], scalar=w12[:, tt, k:k + 1], in1=xres[:, tt, :], op0=ALU.mult,
                op1=ALU.add),
                reads=[kgb, key("w12", tt), key("xres", tt)], writes=[key("xres", tt)])

    hTf = hT[:].rearrange("p a b -> p (a b)").bitcast(F32)
    t3 = [dict(w1=w1[:], kw1=key("w1"), junk=nlo[:], kjunk=key("nlo"), nhi=nhi[:], knhi=key("nhi"), sm0=0,
               pt=pt_[:], kpt=key("pt"), pb=pb[:], kpb=key("pb"), ptr=pT[:], kptr=key("pT"),
               G=A, kG="A", Bk=B[0], kB=key("B", 0)),
          dict(w1=hTf, kw1=key("hT"), junk=ysb[1][:], kjunk=key("ysb", 1), nhi=ysb[0][:], knhi=key("ysb", 0), sm0=4,
               pt=sgt[0][:, 0:256], kpt=key("sgt", 0), pb=sgt[1][:].bitcast(BF16)[:, 0:256], kpb=key("sgt", 1),
               ptr=misc[:].bitcast(BF16).rearrange("p (k t) -> p k t", k=8), kptr=key("misc"),
               G=Y, kG="Y", Bk=B[1], kB=key("B", 1))]

    def tile_ops(tt):
        t0 = tt * 128
        par = tt % 2
        q = t3[par]
        c0_ = q["sm0"]
        x_ap = xres[:, tt, :]
        kx = key("xres", tt)
        wo = w2 if par == 0 else w3
        kwo = 'w2' if par == 0 else 'XT'
        P.op(SC, lambda e: e.activation(out=q["junk"], in_=x_ap, func=AF.Square, accum_out=sm[:, c0_:c0_ + 1]),
             reads=[kx], writes=[q["kjunk"], key("sm", c0_)])
        P.mark()
        P.op(SC, lambda e: e.activation(out=sm[:, c0_ + 1:c0_ + 2], in_=sm[:, c0_:c0_ + 1], func=AF.Ln,
                                        scale=1.0 / 1024, bias=eps[:]),
             reads=[key("sm", c0_), key("eps")], writes=[key("sm", c0_ + 1)])
        P.op(SC, lambda e: e.activation(out=sm[:, c0_ + 2:c0_ + 3], in_=sm[:, c0_ + 1:c0_ + 2], func=AF.Exp, scale=-0.5),
             reads=[key("sm", c0_ + 1)], writes=[key("sm", c0_ + 2)])
        P.dma("sync", q["pt"], d["p"][t0:t0 + 128, :], writes=[q["kpt"]])
        P.mark()
        P.op(V, lambda e: e.scalar_tensor_tensor(out=q["w1"], in0=x_ap, scalar=sm[:, c0_ + 2:c0_ + 3], in1=bc1[:],
                                                 op0=ALU.mult, op1=ALU.mult),
             reads=[kx, key("sm", c0_ + 2), key("bc1")], writes=[q["kw1"]])
        P.op(G, lambda e: e.tensor_copy(out=q["pb"], in_=q["pt"]), reads=[q["kpt"]], writes=[q["kpb"]])
        P.mark()
        P.op(SC, lambda e: e.copy(out=q["nhi"], in_=q["w1"]), reads=[q["kw1"]], writes=[q["knhi"]])
        P.mark()
        for kc in range(8):
            P.op(T, lambda e, kc=kc: e.transpose(out=q["ptr"][:, kc, :], in_=q["nhi"][:, kc * 128:(kc + 1) * 128],
                                                 identity=ident_b[:]),
                 reads=[q["knhi"], key("ident_b")], writes=[q["kptr"]])
        P.mark()
        P.op(V, lambda e: e.tensor_copy(out=nT3[par][:], in_=q["ptr"]), reads=[q["kptr"]], writes=[knT3[par]])
        P.mark()
        for kc in range(2):
            P.op(T, lambda e, kc=kc: e.transpose(out=q["ptr"][:, kc, :], in_=q["pb"][:, kc * 128:(kc + 1) * 128],
                                                 identity=ident_b[:]),
                 reads=[q["kpb"], key("ident_b")], writes=[q["kptr"]])
        P.mark()
        P.op(SC, lambda e: e.copy(out=pTsl[par][:], in_=q["ptr"][:, 0:2, :]), reads=[q["kptr"]], writes=[kpTs[par]])
        P.mark()
        for half in range(2):
            for kc in range(8):
                P.op(T, lambda e, kc=kc, half=half: e.matmul(q["G"][half][:], lhsT=nT3[par][:, kc, :],
                                                             rhs=wbig[:, kc, half * 512:(half + 1) * 512],
                                                             start=(kc == 0), stop=(kc == 7)),
                     reads=[knT3[par], key("wbig")], writes=[key(q["kG"], half)])
            P.mark()
            P.op(V, lambda e, half=half: e.tensor_tensor(out=wo[:, half * 512:(half + 1) * 512], in0=q["G"][half][:],
                                                         in1=bc2[:, half * 512:(half + 1) * 512], op=ALU.add),
                 reads=[key(q["kG"], half), key("bc2")], writes=[key(kwo, half)])
            P.mark()
        P.op(SC, lambda e: e.activation(out=wo[:], in_=wo[:], func=AF.Sigmoid), reads=[key(kwo)], writes=[key(kwo)])
        P.mark()
        for half in range(2):
            for kc in range(2):
                P.op(T, lambda e, kc=kc, half=half: e.matmul(q["Bk"][:], lhsT=pTsl[par][:, kc, :],
                                                             rhs=wpp[:, kc, half * 512:(half + 1) * 512],
                                                             start=(kc == 0), stop=(kc == 1)),
                     reads=[kpTs[par], key("wpp")], writes=[q["kB"]])
            P.mark()
            P.op(V, lambda e, half=half: e.tensor_tensor(out=wo[:, half * 512:(half + 1) * 512], in0=q["Bk"][:],
                                                         in1=wo[:, half * 512:(half + 1) * 512], op=ALU.mult),
                 reads=[q["kB"], key(kwo)], writes=[key(kwo, half)])
            P.mark()
        P.op(G, lambda e: e.tensor_tensor(out=wo[:], in0=wo[:], in1=xres[:, tt, :], op=ALU.add),
             reads=[key(kwo), key("xres", tt)], writes=[key(kwo)])
        P.mark()
        P.dma("sync", d["xo"][t0:t0 + 128, :], wo[:], reads=[key(kwo)], writes=[key("xo")])
        if after_tile is not None:
            after_tile(tt, key("xo"))
        P.mark()

    gback(0)
    gback(1)
    for tt in range(0, NTL, 2):
        if tt + 2 < NTL:
            gback(tt + 2)
            gback(tt + 3)
        P.capture()
        tile_ops(tt)
        s0_ = P.end_capture()
        P.capture()
        tile_ops(tt + 1)
        s1_ = P.end_capture()
        P.replay([s0_, s1_])
    return [key("xo")]


def mixer_inputs(I, L, b, g, xb):
    w_in = I["w_in"][L]
    r0 = g * 256
    cols = np.concatenate([np.arange(r0, r0 + 256), 512 + np.arange(r0, r0 + 256), 1024 + np.arange(r0, r0 + 256),
                           1536 + np.arange(r0, r0 + 256), 2048 + np.arange(r0, r0 + 256), 2560 + 4 * g + np.arange(4)])
    win = np.ascontiguousarray(w_in[:, cols])
    pv = np.zeros((128, NPV), np.float32)
    for c in range(2):
        ch = slice(r0 + c * 128, r0 + (c + 1) * 128)
        for j in range(4):
            pv[:, j * 2 + c] = I["conv_w"][L][j, ch]
        pv[:, 8 + c] = I["conv_b"][L][ch]
        pv[:, 10 + c] = I["b_rgate"][L][ch]
        pv[:, 12 + c] = I["b_igate"][L][ch]
        pv[:, 14 + c] = I["lru_lambda"][L][ch]
        pv[:, 16 + c] = I["lru_out_norm"][L][ch]
    pv[:64, 18] = I["q_norm"][L]
    pv[64:, 18] = I["q_norm"][L]
    pv[:64, 19] = I["k_norm"][L]
    pv[64:, 19] = I["k_norm"][L]
    for hl in range(4):
        pv[:64, 20 + hl] = I["att_out_norm"][L][(4 * g + hl) * 64:(4 * g + hl + 1) * 64]
    wr_bd = np.zeros((2, 128, 128), np.float32)
    wi_bd = np.zeros((2, 128, 128), np.float32)
    for c in range(2):
        for k in range(2):
            blk = 4 * g + 2 * c + k
            wr_bd[c, k * 64:(k + 1) * 64, k * 64:(k + 1) * 64] = I["w_rgate"][L][blk]
            wi_bd[c, k * 64:(k + 1) * 64, k * 64:(k + 1) * 64] = I["w_igate"][L][blk]
    return dict(xb=np.ascontiguousarray(xb), win=win, mixn=np.ascontiguousarray(I["mix_norm"][L]), pv=pv,
                wr_bd=wr_bd, wi_bd=wi_bd, bf=np.ascontiguousarray(I["b_forget"][L][4 * g:4 * g + 4]))

def token_inputs(I, L, x_tok, yT2, ss2, p_tok):
    return dict(x=np.ascontiguousarray(x_tok),
                p=np.ascontiguousarray(p_tok), wout=I["w_out"][L], ffn_norm=I["ffn_norm"][L], ple_norm=I["ple_norm"][L],
                b_ple=I["b_ple_gate"][L], wrt=np.ascontiguousarray(np.concatenate([I["w_group"][L], I["w_router"][L]], 1)),
                brt=np.concatenate([I["b_group"][L], I["b_router"][L]]), weg=I["w_exp_gate"][L], weu=I["w_exp_up"][L],
                wed=I["w_exp_down"][L], wpg=I["w_ple_gate"][L], wpp=I["w_ple_proj"][L])


NT_CORE = 2048
RG = [[0, 1], [2, 3], [4, 5], [6, 7]]
MIX_IN = [("win", [1024, 1284]), ("mixn", [1024]), ("pv", [128, NPV]), ("wr_bd", [2, 128, 128]), ("wi_bd", [2, 128, 128]), ("bf", [4])]
TOK_IN = [("p", [NT_CORE, 256]), ("wout", [1024, 1024]), ("ffn_norm", [1024]), ("ple_norm", [1024]), ("b_ple", [1024]),
          ("wrt", [1024, 36]), ("brt", [36]), ("weg", [32, 1024, 512]), ("weu", [32, 1024, 512]), ("wed", [32, 512, 1024]),
          ("wpg", [1024, 1024]), ("wpp", [256, 1024])]


def build_fused(nlayers=2):
    nc = bass.Bass("TRN2", target_bir_lowering=False)
    ext = lambda name, shape, dt=F32: nc.dram_tensor(name, shape, dt, kind="ExternalInput").ap()
    xb = ext("xb", [4096, 1024])
    xh = ext("xh", [NT_CORE, 1024])
    idx_y4 = ext("idx_y4", [128, 32], I32)
    idx_s = ext("idx_s", [128, 2 * 16], I32)
    out = nc.dram_tensor("out", [NT_CORE, 1024], F32, kind="ExternalOutput").ap()
    sems = Sems(nc)
    x_tile, x_half = None, xh
    for L in range(nlayers):
        dm = {n: ext(f"{n}_{L}", sh) for n, sh in MIX_IN}
        dm["xb"] = xb
        if x_tile is not None:
            dm["xb_tile"] = x_tile
        yT32 = nc.dram_tensor(f"yT_{L}", [512, 2048], F32).ap()
        ss = nc.dram_tensor(f"ss_{L}", [4096, 2], F32).ap()
        yTg32 = nc.dram_tensor(f"yTg_{L}", [1024, 2048], F32).ap()
        ssg = nc.dram_tensor(f"ssg_{L}", [2 * 4096, 2], F32).ap()
        dm["yT"], dm["ss"] = yT32.bitcast(BF16), ss
        P = Prog(nc, sems)
        outs = mixer_phase(P, nc, dm, pfx=f"m{L}")
        for k in range(2):
            P.op("gpsimd", lambda e, k=k: e.collective_compute(
                "AllGather", ALU.bypass, replica_groups=RG, ins=[yT32[k * 256:(k + 1) * 256, :]],
                outs=[yTg32[k * 512:(k + 1) * 512, :]]), reads=outs, writes=[(f"yTg{L}", k)], cc=True)
        P.op("gpsimd", lambda e: e.collective_compute("AllGather", ALU.bypass, replica_groups=RG, ins=[ss[:, :]],
                                                      outs=[ssg[:, :]]),
             reads=outs, writes=[f"ssg{L}"], cc=True)
        P.emit()
        dt_ = {n: ext(f"{n}_{L}", sh) for n, sh in TOK_IN}
        dt_["n2tab"] = nc.dram_tensor(f"n2tab_{L}", [NT_CORE, 1024], BF16).ap()
        dt_["tab"] = nc.dram_tensor(f"tab_{L}", [NSLOT + 128, TABW], I32).ap()
        dt_["ytab"] = nc.dram_tensor(f"ytab_{L}", [NSLOT + 128, 1024], BF16).ap()
        dt_.update(x=x_half, yTg4=yTg32.bitcast(BF16).rearrange("r (b t) -> (r b) t", t=512), ssg=ssg, idx_y4=idx_y4,
                   idx_s=idx_s)
        last = (L == nlayers - 1)
        xo = out if last else nc.dram_tensor(f"xo_{L}", [NT_CORE, 1024], F32).ap()
        dt_["xo"] = xo
        P = Prog(nc, sems)
        if last:
            outs = token_phase(P, nc, dt_, NT=NT_CORE, pfx=f"t{L}", gather=True)
            P.finish_wait("sync", outs)
        else:
            xg = nc.dram_tensor(f"xg_{L}", [4096, 1024], F32).ap()

            def after_tile(tt, kxo, P=P, xo=xo, xg=xg, L=L):
                if tt % 4 == 3:
                    k = tt // 4
                    P.op("gpsimd", lambda e: e.collective_compute(
                        "AllGather", ALU.bypass, replica_groups=RG, ins=[xo[k * 512:(k + 1) * 512, :]],
                        outs=[xg[k * 1024:(k + 1) * 1024, :]]), reads=[kxo], writes=[(f"xg{L}", k)], cc=True)
            outs = token_phase(P, nc, dt_, NT=NT_CORE, pfx=f"t{L}", gather=True, after_tile=after_tile)

            def x_tile(tt, xg=xg):
                s0 = tt * 128
                h, k, i = s0 // 2048, (s0 % 2048) // 512, s0 % 512
                r0 = k * 1024 + h * 512 + i
                return xg[r0:r0 + 128, :]
            x_half = xo
        P.emit()
    sems.close()
    return nc

def _core_inputs(I, c, nlayers=2):
    b, r = c // 2, c % 2
    sl = slice(r * NT_CORE, (r + 1) * NT_CORE)
    m = dict(xb=np.ascontiguousarray(I["x"][b]), xh=np.ascontiguousarray(I["x"][b, sl]))
    p_ = np.arange(128, dtype=np.int64)
    iy = np.zeros((128, 32), np.int64)
    isx = np.zeros((128, 2 * 16), np.int64)
    for g in range(2):
        for cc in range(4):
            k = cc // 2
            for grp in range(4):
                iy[:, (g * 4 + cc) * 4 + grp] = (k * 512 + g * 256 + (cc % 2) * 128 + p_) * 8 + r * 4 + grp
        for tt in range(16):
            isx[:, g * 16 + tt] = g * 4096 + r * 2048 + tt * 128 + p_
    m["idx_y4"] = iy.astype(np.int32)
    m["idx_s"] = isx.astype(np.int32)
    for L in range(nlayers):
        mi = mixer_inputs(I, L, b, r, I["x"][b])
        for n, _ in MIX_IN:
            m[f"{n}_{L}"] = np.ascontiguousarray(mi[n])
        ti = token_inputs(I, L, I["x"][b, sl], None, None, I["p"][L][b][sl])
        for n, _ in TOK_IN:
            m[f"{n}_{L}"] = np.ascontiguousarray(ti[n])
    return m


def kernel(**inputs):
    I = {k: np.asarray(v) for k, v in inputs.items()}
    cores = list(range(8))
    nc = build_fused(2)
    maps = [_core_inputs(I, c) for c in cores]
    res = run_bass_kernel_spmd(nc, maps, core_ids=cores)
    out = np.empty(I["x"].shape, np.float32)
    for c in cores:
        b, r = c // 2, c % 2
        out[b, r * NT_CORE:(r + 1) * NT_CORE] = np.asarray(res.results[c]["out"])
    return out
```
